# Optimizing a Trainium2 kernel written in Bass

```python
import math
import jax, jax.numpy as jnp
from jax import lax
import numpy as np

D_MODEL = 1024
BATCH = 8
SEQ = 2048
DEPTH = 4

N_MIXERS = 3
N_RET = (DEPTH + 2) // 3
N_GMLP = (DEPTH + 1) // 3
N_CONV = DEPTH // 3

RET_HEADS = 4
RET_DK = D_MODEL // RET_HEADS
RET_DV = 2 * D_MODEL // RET_HEADS
RET_IN = 2 * D_MODEL + 2 * (2 * D_MODEL)
RET_CHUNK = 128
ROPE_BASE = 10000.0

GMLP_D_FFN = 6 * D_MODEL
GMLP_HALF = GMLP_D_FFN // 2
GMLP_GROUPS = 4
GMLP_GROUP_DIM = GMLP_HALF // GMLP_GROUPS
GMLP_CHUNK = 128

CONV_WIDTH = 3

MEM_LEN = 256
XA_HEADS = 4
XA_DH = D_MODEL // XA_HEADS

D_FF = 4 * D_MODEL

NORM_EPS = 1e-6
GN_EPS = 1e-5

kernel_name = "hybrid_retention_gmlp_shortconv_trunk"


def rms_norm(x, g):
    xf = x.astype(jnp.float32)
    y = xf * lax.rsqrt(jnp.mean(xf * xf, axis=-1, keepdims=True) + NORM_EPS)
    return (y * g.astype(jnp.float32)).astype(x.dtype)


def layer_norm(x, g, b):
    xf = x.astype(jnp.float32)
    mu = jnp.mean(xf, axis=-1, keepdims=True)
    var = jnp.mean(jnp.square(xf - mu), axis=-1, keepdims=True)
    y = (xf - mu) * lax.rsqrt(var + GN_EPS)
    return (y * g.astype(jnp.float32) + b.astype(jnp.float32)).astype(x.dtype)


def rotary(t, positions):
    half = t.shape[-1] // 2
    inv_freq = ROPE_BASE ** (-jnp.arange(half, dtype=jnp.float32) / half)
    ang = positions.astype(jnp.float32)[..., None] * inv_freq
    cos = jnp.cos(ang)[:, :, None, :]
    sin = jnp.sin(ang)[:, :, None, :]
    tf = t.astype(jnp.float32)
    t1, t2 = tf[..., :half], tf[..., half:]
    out = jnp.concatenate([t1 * cos - t2 * sin, t2 * cos + t1 * sin], axis=-1)
    return out.astype(t.dtype)


def retention_mixer(xn, positions, w_in, gn_g, w_out):
    B, S, _ = xn.shape
    proj = xn @ w_in
    q, k, v, g = jnp.split(proj, [D_MODEL, 2 * D_MODEL, 4 * D_MODEL], axis=-1)
    q = rotary(q.reshape(B, S, RET_HEADS, RET_DK), positions)
    k = rotary(k.reshape(B, S, RET_HEADS, RET_DK), positions) * (RET_DK ** -0.5)
    v = v.reshape(B, S, RET_HEADS, RET_DV)
    nc = S // RET_CHUNK

    def to_chunks(t):
        return t.reshape(B, nc, RET_CHUNK, RET_HEADS, t.shape[-1]).transpose(1, 0, 3, 2, 4)

    qc, kc, vc = to_chunks(q), to_chunks(k), to_chunks(v)

    log_g = jnp.log(1.0 - 2.0 ** (-5.0 - jnp.arange(RET_HEADS, dtype=jnp.float32)))
    idx = jnp.arange(RET_CHUNK, dtype=jnp.float32)
    rel = idx[:, None] - idx[None, :]
    intra = jnp.where(rel[None] >= 0, jnp.exp(rel[None] * log_g[:, None, None]), 0.0)
    xi = jnp.exp((idx[None] + 1.0) * log_g[:, None])
    zeta = jnp.exp((RET_CHUNK - 1.0 - idx[None]) * log_g[:, None])
    chunk_decay = jnp.exp(RET_CHUNK * log_g)

    def step(R, qkv):
        qi, ki, vi = qkv
        s = jnp.einsum('bhtd,bhsd->bhts', qi, ki) * intra[None]
        o_intra = jnp.einsum('bhts,bhse->bhte', s, vi)
        o_inter = jnp.einsum('bhtd,bhde->bhte', qi, R) * xi[None, :, :, None]
        R_new = R * chunk_decay[None, :, None, None] + jnp.einsum(
            'bhsd,bhse->bhde', ki * zeta[None, :, :, None], vi)
        return R_new, o_intra + o_inter

    R0 = jnp.zeros((B, RET_HEADS, RET_DK, RET_DV), jnp.float32)
    _, o = lax.scan(step, R0, (qc, kc, vc))
    o = o.transpose(1, 0, 3, 2, 4).reshape(B, S, RET_HEADS, RET_DV)
    mu = jnp.mean(o, axis=-1, keepdims=True)
    var = jnp.mean(jnp.square(o - mu), axis=-1, keepdims=True)
    o = (o - mu) * lax.rsqrt(var + GN_EPS)
    o = (o.reshape(B, S, 2 * D_MODEL) * gn_g.astype(jnp.float32)).astype(xn.dtype)
    return (jax.nn.silu(g) * o) @ w_out


def gmlp_mixer(xn, w_in, ln_g, ln_b, w_s, b_s, w_out):
    B, S, _ = xn.shape
    z = jax.nn.gelu(xn @ w_in, approximate=False)
    u, v = jnp.split(z, 2, axis=-1)
    v = layer_norm(v, ln_g, ln_b)
    nc = S // GMLP_CHUNK
    v = v.reshape(B, nc, GMLP_CHUNK, GMLP_GROUPS, GMLP_GROUP_DIM)
    mask = jnp.tril(jnp.ones((GMLP_CHUNK, GMLP_CHUNK), dtype=w_s.dtype))
    sv = jnp.einsum('gts,bnsgc->bntgc', w_s * mask[None], v) + b_s.T[None, None, :, :, None]
    return (u * sv.reshape(B, S, GMLP_HALF)) @ w_out


def short_conv_mixer(xn, w_in, conv_k, w_out):
    b_gate, c_gate, h = jnp.split(xn @ w_in, 3, axis=-1)
    z = lax.conv_general_dilated(
        c_gate * h, conv_k[:, None, :].astype(h.dtype),
        window_strides=(1,), padding=[(CONV_WIDTH - 1, 0)],
        dimension_numbers=('NWC', 'WIO', 'NWC'), feature_group_count=D_MODEL)
    return (b_gate * z) @ w_out


def memory_cross_attention(xn, mem_n, w_q, w_kv, w_o):
    B, S, _ = xn.shape
    q = (xn @ w_q).reshape(B, S, XA_HEADS, XA_DH)
    k, v = jnp.split(mem_n @ w_kv, 2, axis=-1)
    k = k.reshape(B, MEM_LEN, XA_HEADS, XA_DH)
    v = v.reshape(B, MEM_LEN, XA_HEADS, XA_DH)
    s = jnp.einsum('bshd,bmhd->bhsm', q, k).astype(jnp.float32) * (XA_DH ** -0.5)
    p = jax.nn.softmax(s, axis=-1).astype(v.dtype)
    o = jnp.einsum('bhsm,bmhd->bshd', p, v).reshape(B, S, XA_HEADS * XA_DH)
    return o @ w_o


def sq_relu_mlp(xn, w1, w2):
    return jnp.square(jax.nn.relu(xn @ w1)) @ w2


def setup_inputs(seed: int = 0) -> dict:
    key = jax.random.key(seed)
    ks = iter(jax.random.split(key, 32))

    def w(shape, fan_in):
        return jax.random.normal(next(ks), shape, jnp.float32) * (fan_in ** -0.5)

    def gain(shape):
        return 1.0 + 0.02 * jax.random.normal(next(ks), shape, jnp.float32)

    x = jax.random.normal(next(ks), (BATCH, SEQ, D_MODEL), jnp.float32)
    mem = jax.random.normal(next(ks), (BATCH, MEM_LEN, D_MODEL), jnp.float32)
    offs = jax.random.randint(next(ks), (BATCH, 1), 0, 4096, dtype=jnp.int32)
    positions = jnp.arange(SEQ, dtype=jnp.int32)[None, :] + offs
    return {
        "x": x,
        "mem": mem,
        "positions": positions,
        "norm_mix_g": gain((DEPTH, D_MODEL)),
        "norm_xa_g": gain((DEPTH, D_MODEL)),
        "norm_mem_g": gain((DEPTH, D_MODEL)),
        "xa_w_q": w((DEPTH, D_MODEL, XA_HEADS * XA_DH), D_MODEL),
        "xa_w_kv": w((DEPTH, D_MODEL, 2 * XA_HEADS * XA_DH), D_MODEL),
        "xa_w_o": w((DEPTH, XA_HEADS * XA_DH, D_MODEL), XA_HEADS * XA_DH),
        "norm_ffn_g": gain((DEPTH, D_MODEL)),
        "ffn_w1": w((DEPTH, D_MODEL, D_FF), D_MODEL),
        "ffn_w2": w((DEPTH, D_FF, D_MODEL), D_FF),
        "ret_w_in": w((N_RET, D_MODEL, RET_IN), D_MODEL),
        "ret_gn_g": gain((N_RET, 2 * D_MODEL)),
        "ret_w_out": w((N_RET, 2 * D_MODEL, D_MODEL), 2 * D_MODEL),
        "gmlp_w_in": w((N_GMLP, D_MODEL, GMLP_D_FFN), D_MODEL),
        "gmlp_ln_g": gain((N_GMLP, GMLP_HALF)),
        "gmlp_ln_b": 0.02 * jax.random.normal(next(ks), (N_GMLP, GMLP_HALF), jnp.float32),
        "gmlp_w_s": w((N_GMLP, GMLP_GROUPS, GMLP_CHUNK, GMLP_CHUNK), GMLP_CHUNK),
        "gmlp_b_s": gain((N_GMLP, GMLP_GROUPS, GMLP_CHUNK)),
        "gmlp_w_out": w((N_GMLP, GMLP_HALF, D_MODEL), GMLP_HALF),
        "conv_w_in": w((N_CONV, D_MODEL, 3 * D_MODEL), D_MODEL),
        "conv_k": w((N_CONV, CONV_WIDTH, D_MODEL), CONV_WIDTH),
        "conv_w_out": w((N_CONV, D_MODEL, D_MODEL), D_MODEL),
        "norm_f_g": gain((D_MODEL,)),
    }


def reference(x, mem, positions, norm_mix_g, norm_xa_g, norm_mem_g, xa_w_q, xa_w_kv, xa_w_o,
              norm_ffn_g, ffn_w1, ffn_w2, ret_w_in, ret_gn_g, ret_w_out,
              gmlp_w_in, gmlp_ln_g, gmlp_ln_b, gmlp_w_s, gmlp_b_s, gmlp_w_out,
              conv_w_in, conv_k, conv_w_out, norm_f_g):
    h = x
    for i in range(DEPTH):
        kind, j = i % N_MIXERS, i // N_MIXERS
        hn = rms_norm(h, norm_mix_g[i])
        if kind == 0:
            mix = retention_mixer(hn, positions, ret_w_in[j], ret_gn_g[j], ret_w_out[j])
        elif kind == 1:
            mix = gmlp_mixer(hn, gmlp_w_in[j], gmlp_ln_g[j], gmlp_ln_b[j],
                             gmlp_w_s[j], gmlp_b_s[j], gmlp_w_out[j])
        else:
            mix = short_conv_mixer(hn, conv_w_in[j], conv_k[j], conv_w_out[j])
        h = h + mix
        h = h + memory_cross_attention(rms_norm(h, norm_xa_g[i]), rms_norm(mem, norm_mem_g[i]),
                                       xa_w_q[i], xa_w_kv[i], xa_w_o[i])
        h = h + sq_relu_mlp(rms_norm(h, norm_ffn_g[i]), ffn_w1[i], ffn_w2[i])
    return rms_norm(h, norm_f_g)
```

```python
import math
import numpy as np
import concourse.bass as bass
import concourse.mybir as mybir
from concourse.bass_utils import run_bass_kernel_spmd

F32 = mybir.dt.float32
BF16 = mybir.dt.bfloat16
I32 = mybir.dt.int32
U8 = mybir.dt.uint8
AF = mybir.ActivationFunctionType
ALU = mybir.AluOpType

D = 1024
S = 2048
DEPTH = 4
KC = 8
NTB = 4
TB = 512
MEM = 256
NSLOT = 5
SLOT_BYTES = 8192
RET_HEADS = 4
NORM_EPS = 1e-6
GN_EPS = 1e-5

_ESZ = {F32: 4, BF16: 2, I32: 4, U8: 1}


def _prod(xs):
    r = 1
    for x in xs:
        r *= int(x)
    return r


class Op:
    __slots__ = ("eng", "fn", "deps", "waits", "inc", "dma", "idx", "dmaval")

    def __init__(self, eng, fn, dma):
        self.eng = eng
        self.fn = fn
        self.deps = []
        self.waits = []
        self.inc = False
        self.dma = dma
        self.idx = -1
        self.dmaval = 0


class Prog:
    ENGS = ("pe", "act", "dve", "pool", "sp")

    def __init__(self, nc, sb, ps, dry=False, sched=None):
        self.nc = nc
        self.sb = sb
        self.ps = ps
        self.dry = dry
        self.ops = {e: [] for e in self.ENGS}
        self.rec = {}
        self.wm = {e: {} for e in self.ENGS}
        self.dmacount = {}
        self.nbank = 0
        self.pools = {}
        self.sched = sched if sched is not None else []
        self.wreq = 0
        self.wissued = 0
        self.free_slots = list(range(NSLOT))
        self.tile_slot = {}
        self.sb_off = 0

    def sbv(self, off, shape, dt):
        n = _prod(shape) * _ESZ[dt]
        v = self.sb[:, off:off + n].bitcast(dt)
        if len(shape) == 2:
            v = v.rearrange("p (a b) -> p a b", a=shape[0])
        elif len(shape) == 3:
            v = v.rearrange("p (a b c) -> p a b c", a=shape[0], b=shape[1])
        return v

    def bank(self, dt=F32, pool=None):
        if pool is not None and pool in self.pools:
            lst = self.pools[pool]
            b = lst[0]
            lst.append(lst.pop(0))
        else:
            b = self.nbank
            self.nbank = (self.nbank + 1) % 8
        return self.ps[:, b, :].bitcast(dt)

    @staticmethod
    def region(ap):
        t = ap.tensor
        esz = _ESZ[ap.dtype]
        row = _prod(t.shape[1:])
        off = int(ap.offset)
        p0 = off // row
        f0 = off % row
        a = ap.ap
        npart = a[0][1]
        lo = f0
        hi = f0
        for step, cnt in a[1:]:
            if step >= 0:
                hi += step * (cnt - 1)
            else:
                lo += step * (cnt - 1)
        blo, bhi = lo * esz, (hi + 1) * esz
        if t.name == "ps":
            blo = (blo // 2048) * 2048
            bhi = ((bhi + 2047) // 2048) * 2048
        return t.name, p0, p0 + npart, blo, bhi

    def add(self, eng, fn, reads=(), writes=(), dma=None):
        if self.dry:
            return
        op = Op(eng, fn, dma)
        lst = self.ops[eng]
        op.idx = len(lst)
        lst.append(op)
        if dma is not None:
            self.dmacount.setdefault(dma, 0)
            op.dmaval = self.dmacount[dma] + 16
            tok = ("dma", dma)
        else:
            tok = ("eng", eng)
        deps = {}

        myval = op.dmaval if dma is not None else op.idx

        def need(t, v):
            if t == tok and v == myval:
                return
            if t[0] == "dma":
                v = self.dmacount[t[1]]
            if deps.get(t, -1) < v:
                deps[t] = v

        for ap in reads:
            name, p0, p1, lo, hi = self.region(ap)
            for r in self.rec.get(name, ()):
                if r[0] < p1 and p0 < r[1] and r[2] < hi and lo < r[3]:
                    if r[4] is not None:
                        need(*r[4])
                    r[5][tok] = myval
        for ap in writes:
            name, p0, p1, lo, hi = self.region(ap)
            lst2 = self.rec.setdefault(name, [])
            keep = []
            for r in lst2:
                if r[0] < p1 and p0 < r[1] and r[2] < hi and lo < r[3]:
                    if r[4] is not None:
                        need(*r[4])
                    for t, v in r[5].items():
                        need(t, v)
                    if p0 <= r[0] and r[1] <= p1 and lo <= r[2] and r[3] <= hi:
                        continue
                keep.append(r)
            keep.append([p0, p1, lo, hi, (tok, myval), {}])
            self.rec[name] = keep
        if dma is not None:
            self.dmacount[dma] = op.dmaval
        wm = self.wm[eng]
        for t, v in deps.items():
            if t == tok and dma is None and eng == "pe":
                continue
            if t == tok and dma is None and v >= op.idx:
                continue
            if wm.get(t, -1) >= v:
                continue
            wm[t] = v
            op.waits.append((t, v))
            if t[0] == "eng":
                self.ops[t[1]][v].inc = True

    def wtile(self, pieces):
        i = self.wreq
        self.wreq += 1
        if self.dry:
            self.sched.append(pieces)
            return 0
        self._issue()
        if i not in self.tile_slot:
            raise RuntimeError("weight ring too small at tile %d" % i)
        return i

    def wview(self, i, a, b, eoff=0):
        if self.dry:
            return None
        s = self.tile_slot[i]
        return self.sbv(self.WR_OFF + s * SLOT_BYTES + eoff * 2, (a, b), BF16)

    def wrelease(self, i):
        if self.dry:
            return
        self.free_slots.append(self.tile_slot[i])
        self._issue()

    def _issue(self):
        while self.free_slots and self.wissued < len(self.sched):
            s = self.free_slots.pop(0)
            t = self.wissued
            self.wissued += 1
            self.tile_slot[t] = s
            for (src, eoff, a, b) in self.sched[t]:
                dst = self.sbv(self.WR_OFF + s * SLOT_BYTES + eoff * 2, (a, b), BF16)
                self.add("pool", (lambda e, dst=dst, src=src: e.dma_start(out=dst, in_=src)),
                         writes=[dst], dma="w%d" % s)

    def emit(self, final_dma_keys=("out",)):
        nc = self.nc
        import contextlib
        with contextlib.ExitStack() as st:
            esem = {e: st.enter_context(nc.semaphore("s_" + e)) for e in self.ENGS}
            dsem = {k: st.enter_context(nc.semaphore("d_" + k)) for k in self.dmacount}
            incval = {}
            for e in self.ENGS:
                c = 0
                vals = []
                for op in self.ops[e]:
                    if op.inc:
                        c += 1
                    vals.append(c)
                incval[e] = vals
            block = st.enter_context(nc.Block())

            def run(engname, eng):
                for op in self.ops[engname]:
                    for (t, v) in op.waits:
                        if t[0] == "eng":
                            eng.wait_ge(esem[t[1]], incval[t[1]][v])
                        else:
                            eng.wait_ge(dsem[t[1]], v)
                    ins = op.fn(eng)
                    if op.dma is not None:
                        ins.then_inc(dsem[op.dma], 16)
                    elif op.inc:
                        ins.then_inc(esem[engname], 1)
                if engname == "sp":
                    for k in final_dma_keys:
                        eng.wait_ge(dsem[k], self.dmacount[k])

            block.tensor(lambda e: run("pe", e))
            block.scalar(lambda e: run("act", e))
            block.vector(lambda e: run("dve", e))
            block.gpsimd(lambda e: run("pool", e))
            block.sync(lambda e: run("sp", e))


class Builder:
    def __init__(self, P, T, cfg):
        self.P = P
        self.T = T
        self.cfg = cfg
        P.WR_OFF = 0
        o = NSLOT * SLOT_BYTES
        self.HT = P.sbv(o, (KC, S), F32) if not P.dry else None
        self.HT_OFF = o
        o += KC * S * 4
        self.XN_OFF = o
        o += KC * S * 2
        self.PAR_OFF = o
        o += cfg["npar"] * 4
        self.ONES_OFF = o
        o += 256
        self.ID_OFF = o
        o += 256
        self.ZC_OFF = o
        o += 64
        self.SCR = o
        self.SCR_END = 212480
        if not P.dry:
            self.XN = P.sbv(self.XN_OFF, (KC, S), BF16)
            self.PAR = P.sbv(self.PAR_OFF, (cfg["npar"],), F32)
            self.ONES = P.sbv(self.ONES_OFF, (128,), BF16)
            self.IDENT = P.sbv(self.ID_OFF, (128,), BF16)

    def salloc_reset(self):
        self._so = self.SCR

    def salloc(self, shape, dt):
        n = _prod(shape) * _ESZ[dt]
        n = (n + 63) // 64 * 64
        off = self._so
        self._so += n
        assert self._so <= self.SCR_END, ("scratch overflow", self._so)
        if self.P.dry:
            return None
        return self.P.sbv(off, shape, dt)

    def par(self, col, n=1):
        return self.PAR[:, col:col + n]

    def prologue(self):
        P, T = self.P, self.T
        if P.dry:
            return
        xT = T["xT"].rearrange("(c p) t -> p c t", p=128)
        for c in range(KC):
            dst = self.HT[:, c, :]
            P.add("sp", (lambda e, dst=dst, c=c: e.dma_start(out=dst, in_=xT[:, c, :])), writes=[dst], dma="in")
        P.add("sp", lambda e: e.dma_start(out=self.PAR, in_=T["params"][:, :]), writes=[self.PAR], dma="in")
        P.add("sp", lambda e: e.dma_start(out=self.ONES, in_=T["ones_bf"][:, :]), writes=[self.ONES], dma="in")
        P.add("sp", lambda e: e.dma_start(out=self.IDENT, in_=T["ident_bf"][:, :]), writes=[self.IDENT], dma="in")

    def rmsnorm(self, gcol, inplace=False):
        P = self.P
        self.salloc_reset()
        sq = self.salloc((KC, TB), BF16)
        rs = [self.salloc((TB,), F32) for _ in range(2)]
        rr = [self.salloc((TB,), F32) for _ in range(2)]
        if P.dry:
            return
        for tb in range(NTB):
            ts = slice(tb * TB, (tb + 1) * TB)
            pb = P.bank()
            for c in range(KC):
                src = self.HT[:, c, ts]
                dst = sq[:, c, :]
                P.add("act", (lambda e, dst=dst, src=src: e.activation(out=dst, in_=src, func=AF.Square)),
                      reads=[src], writes=[dst])
                P.add("pe", (lambda e, c=c, dst=dst, pb=pb: e.matmul(pb, lhsT=self.ONES, rhs=dst, start=(c == 0), stop=(c == KC - 1))),
                      reads=[self.ONES, dst], writes=[pb])
            r1 = rs[tb % 2]
            r2 = rr[tb % 2]
            P.add("act", (lambda e, r1=r1, pb=pb: e.activation(out=r1, in_=pb, func=AF.Sqrt, scale=1.0 / D, bias=self.par(self.cfg["eps_norm"]))),
                  reads=[pb, self.PAR], writes=[r1])
            P.add("dve", (lambda e, r1=r1, r2=r2: e.reciprocal(out=r2, in_=r1)), reads=[r1], writes=[r2])
            for c in range(KC):
                src = self.HT[:, c, ts]
                dst = src if inplace else self.XN[:, c, ts]
                g = self.par(gcol + c)
                P.add("dve", (lambda e, dst=dst, src=src, g=g, r2=r2: e.scalar_tensor_tensor(out=dst, in0=src, scalar=g, in1=r2, op0=ALU.mult, op1=ALU.mult)),
                      reads=[src, r2, self.PAR], writes=[dst])

    def lin_fm(self, wv, nk, nm, rhs_fn, evac_fn, tbs=range(NTB)):
        P = self.P
        for mi in range(nm):
            for tb in tbs:
                pb = P.bank()
                for k in range(nk):
                    lhsT = wv[:, k, mi * 128:(mi + 1) * 128]
                    rhs = rhs_fn(k, tb)
                    P.add("pe", (lambda e, pb=pb, lhsT=lhsT, rhs=rhs, k=k: e.matmul(pb, lhsT=lhsT, rhs=rhs, start=(k == 0), stop=(k == nk - 1))),
                          reads=[lhsT, rhs], writes=[pb])
                evac_fn(pb, mi, tb)

    def add_to_h(self, pb, m, tb):
        P = self.P
        dst = self.HT[:, m, tb * TB:(tb + 1) * TB]
        P.add("dve", (lambda e, dst=dst, pb=pb: e.tensor_tensor(out=dst, in0=pb, in1=dst, op=ALU.add)),
              reads=[pb, dst], writes=[dst])

    def ffn(self, l):
        P, T = self.P, self.T
        self.rmsnorm(self.cfg["g_ffn"] + l * KC)
        self.salloc_reset()
        h1 = [self.salloc((4, S), BF16) for _ in range(2)]
        r32 = [self.salloc((TB,), F32) for _ in range(3)]
        w1 = T["ffn_w1"][l].rearrange("(kc p) f -> p kc f", p=128)
        w2 = T["ffn_w2"][l].rearrange("(fc p) m -> p fc m", p=128)
        NF = 8
        cnt = [0]

        def w1_phase(j):
            t = P.wtile([(w1[:, :, j * 512:(j + 1) * 512], 0, KC, 512)])
            if P.dry:
                return
            wv = P.wview(t, KC, 512)
            hb = h1[j % 2]

            def evac(pb, mi, tb):
                r = r32[cnt[0] % 3]
                cnt[0] += 1
                dst = hb[:, mi, tb * TB:(tb + 1) * TB]
                P.add("act", (lambda e, r=r, pb=pb: e.activation(out=r, in_=pb, func=AF.Relu)), reads=[pb], writes=[r])
                P.add("act", (lambda e, r=r, dst=dst: e.activation(out=dst, in_=r, func=AF.Square)), reads=[r], writes=[dst])
            self.lin_fm(wv, KC, 4, lambda k, tb: self.XN[:, k, tb * TB:(tb + 1) * TB], evac)
            P.wrelease(t)

        def w2_phase(j):
            t = P.wtile([(w2[:, j * 4:(j + 1) * 4, :], 0, 4, D)])
            if P.dry:
                return
            wv = P.wview(t, 4, D)
            hb = h1[j % 2]
            self.lin_fm(wv, 4, KC, lambda k, tb: hb[:, k, tb * TB:(tb + 1) * TB], self.add_to_h)
            P.wrelease(t)

        w1_phase(0)
        for j in range(NF):
            if j + 1 < NF:
                w1_phase(j + 1)
            w2_phase(j)

    def act_copy(self, dst, src, scale=None, func=None):
        P = self.P
        f = func if func is not None else AF.Copy
        if scale is None:
            P.add("act", (lambda e, dst=dst, src=src, f=f: e.activation(out=dst, in_=src, func=f)), reads=[src], writes=[dst])
        else:
            P.add("act", (lambda e, dst=dst, src=src, f=f, scale=scale: e.activation(out=dst, in_=src, func=f, scale=scale)), reads=[src], writes=[dst])

    def wtiles_cols(self, w, col0, ntiles, width=512):
        P = self.P
        wv = w.rearrange("(kc p) n -> p kc n", p=128)
        return [P.wtile([(wv[:, :, col0 + i * width: col0 + (i + 1) * width], 0, KC, width)]) for i in range(ntiles)]

    def xattn(self, l):
        P, T = self.P, self.T
        self.rmsnorm(self.cfg["g_xa"] + l * KC)
        self.salloc_reset()
        qT = self.salloc((KC, S), BF16)
        memT = self.salloc((KC, MEM), F32)
        msq = self.salloc((KC, MEM), BF16)
        mrs = self.salloc((MEM,), F32)
        mrr = self.salloc((MEM,), F32)
        memn = self.salloc((KC, MEM), BF16)
        kT = self.salloc((KC, MEM), BF16)
        vtok = self.salloc((2, D), BF16)
        pT = [self.salloc((2, TB), BF16) for _ in range(2)]
        rden = [self.salloc((TB,), F32) for _ in range(2)]
        wkv = T["xa_w_kv"][l]
        if P.dry:
            self.wtiles_cols(wkv, 0, 2)
            self.wtiles_cols(wkv, D, 2)
            self.wtiles_cols(T["xa_w_q"][l], 0, 2)
            self.wtiles_cols(T["xa_w_o"][l], 0, 2)
            return
        XN = self.XN
        mT = T["memT"].rearrange("(c p) t -> p c t", p=128)
        P.add("sp", lambda e: e.dma_start(out=memT, in_=mT), writes=[memT], dma="mem")
        pb = P.bank()
        pbm = pb[:, 0:MEM]
        for c in range(KC):
            self.act_copy(msq[:, c, :], memT[:, c, :], func=AF.Square)
            P.add("pe", (lambda e, c=c: e.matmul(pbm, lhsT=self.ONES, rhs=msq[:, c, :], start=(c == 0), stop=(c == KC - 1))),
                  reads=[self.ONES, msq[:, c, :]], writes=[pbm])
        P.add("act", lambda e: e.activation(out=mrs, in_=pbm, func=AF.Sqrt, scale=1.0 / D, bias=self.par(self.cfg["eps_norm"])),
              reads=[pbm, self.PAR], writes=[mrs])
        P.add("dve", lambda e: e.reciprocal(out=mrr, in_=mrs), reads=[mrs], writes=[mrr])
        for c in range(KC):
            g = self.par(self.cfg["g_mem"] + l * KC + c)
            P.add("dve", (lambda e, c=c, g=g: e.scalar_tensor_tensor(out=memn[:, c, :], in0=memT[:, c, :], scalar=g, in1=mrr, op0=ALU.mult, op1=ALU.mult)),
                  reads=[memT[:, c, :], mrr, self.PAR], writes=[memn[:, c, :]])
        for i in range(2):
            t = self.wtiles_cols(wkv, i * 512, 1)[0]
            wv = P.wview(t, KC, 512)
            for mi in range(4):
                pb = P.bank()
                pbm = pb[:, 0:MEM]
                for k in range(KC):
                    lhsT = wv[:, k, mi * 128:(mi + 1) * 128]
                    P.add("pe", (lambda e, pbm=pbm, lhsT=lhsT, k=k: e.matmul(pbm, lhsT=lhsT, rhs=memn[:, k, :], start=(k == 0), stop=(k == KC - 1))),
                          reads=[lhsT, memn[:, k, :]], writes=[pbm])
                self.act_copy(kT[:, i * 4 + mi, :], pbm, scale=1.0 / 16.0)
            P.wrelease(t)
        for i in range(2):
            t = self.wtiles_cols(wkv, D + i * 512, 1)[0]
            wv = P.wview(t, KC, 512)
            for mt in range(2):
                pb = P.bank()
                for k in range(KC):
                    lhsT = memn[:, k, mt * 128:(mt + 1) * 128]
                    rhs = wv[:, k, :]
                    P.add("pe", (lambda e, pb=pb, lhsT=lhsT, rhs=rhs, k=k: e.matmul(pb, lhsT=lhsT, rhs=rhs, start=(k == 0), stop=(k == KC - 1))),
                          reads=[lhsT, rhs], writes=[pb])
                self.act_copy(vtok[:, mt, i * 512:(i + 1) * 512], pb)
            P.wrelease(t)
        for i in range(2):
            t = self.wtiles_cols(T["xa_w_q"][l], i * 512, 1)[0]
            wv = P.wview(t, KC, 512)
            self.lin_fm(wv, KC, 4, lambda k, tb: XN[:, k, tb * TB:(tb + 1) * TB],
                        lambda pb, mi, tb, i=i: self.act_copy(qT[:, i * 4 + mi, tb * TB:(tb + 1) * TB], pb))
            P.wrelease(t)
        it = 0
        for h in range(4):
            for tb in range(NTB):
                ts = slice(tb * TB, (tb + 1) * TB)
                pt = pT[it % 2]
                rd = rden[it % 2]
                it += 1
                for mt in range(2):
                    pb = P.bank()
                    for dc in range(2):
                        lhsT = kT[:, h * 2 + dc, mt * 128:(mt + 1) * 128]
                        rhs = qT[:, h * 2 + dc, ts]
                        P.add("pe", (lambda e, pb=pb, lhsT=lhsT, rhs=rhs, dc=dc: e.matmul(pb, lhsT=lhsT, rhs=rhs, start=(dc == 0), stop=(dc == 1))),
                              reads=[lhsT, rhs], writes=[pb])
                    self.act_copy(pt[:, mt, :], pb, func=AF.Exp)
                pd = P.bank()
                for mt in range(2):
                    rhs = pt[:, mt, :]
                    P.add("pe", (lambda e, pd=pd, rhs=rhs, mt=mt: e.matmul(pd, lhsT=self.ONES, rhs=rhs, start=(mt == 0), stop=(mt == 1))),
                          reads=[self.ONES, rhs], writes=[pd])
                P.add("dve", (lambda e, rd=rd, pd=pd: e.reciprocal(out=rd, in_=pd)), reads=[pd], writes=[rd])
                for dc in range(2):
                    po = P.bank()
                    for mt in range(2):
                        lhsT = vtok[:, mt, h * 256 + dc * 128: h * 256 + (dc + 1) * 128]
                        rhs = pt[:, mt, :]
                        P.add("pe", (lambda e, po=po, lhsT=lhsT, rhs=rhs, mt=mt: e.matmul(po, lhsT=lhsT, rhs=rhs, start=(mt == 0), stop=(mt == 1))),
                              reads=[lhsT, rhs], writes=[po])
                    dst = XN[:, h * 2 + dc, ts]
                    P.add("dve", (lambda e, dst=dst, po=po, rd=rd: e.tensor_tensor(out=dst, in0=po, in1=rd, op=ALU.mult)),
                          reads=[po, rd], writes=[dst])
        for i in range(2):
            t = self.wtiles_cols(T["xa_w_o"][l], i * 512, 1)[0]
            wv = P.wview(t, KC, 512)
            self.lin_fm(wv, KC, 4, lambda k, tb: XN[:, k, tb * TB:(tb + 1) * TB],
                        lambda pb, mi, tb, i=i: self.add_to_h(pb, i * 4 + mi, tb))
            P.wrelease(t)

    def conv(self, l, j):
        P, T = self.P, self.T
        self.rmsnorm(self.cfg["g_mix"] + l * KC)
        self.salloc_reset()
        yT = self.salloc((KC, S), BF16)
        ch = self.salloc((S + 16,), F32)
        bsb = [self.salloc((TB,), F32) for _ in range(2)]
        csb = [self.salloc((TB,), F32) for _ in range(2)]
        zz = [self.salloc((TB,), F32) for _ in range(2)]
        w_in = T["conv_w_in"][j]
        XN = None if P.dry else self.XN
        dbg = self.cfg.get("dbg", "")
        if not P.dry and "nomemset" not in dbg:
            P.add("dve", lambda e: e.tensor_scalar(out=ch[:, 0:2], in0=self.PAR[:, 0:2], scalar1=0.0, scalar2=None, op0=ALU.mult),
                  reads=[self.PAR], writes=[ch[:, 0:2]])
        it = 0
        for g in range(2):
            tb_ = self.wtiles_cols(w_in, 0 * D + g * 512, 1)[0]
            tc_ = self.wtiles_cols(w_in, 1 * D + g * 512, 1)[0]
            th_ = self.wtiles_cols(w_in, 2 * D + g * 512, 1)[0]
            if P.dry:
                continue
            vb, vc, vh = (P.wview(t, KC, 512) for t in (tb_, tc_, th_))
            for fi in range(4):
                fc = g * 4 + fi
                k0 = self.par(self.cfg["conv_k"] + 0 * KC + fc)
                k1 = self.par(self.cfg["conv_k"] + 1 * KC + fc)
                k2 = self.par(self.cfg["conv_k"] + 2 * KC + fc)
                for tb in range(NTB):
                    ts = slice(tb * TB, (tb + 1) * TB)
                    b_, c_, z_ = bsb[it % 2], csb[it % 2], zz[it % 2]
                    it += 1
                    pbs = []
                    for wv in (vb, vc, vh):
                        pb = P.bank()
                        pbs.append(pb)
                        for k in range(KC):
                            lhsT = wv[:, k, fi * 128:(fi + 1) * 128]
                            rhs = XN[:, k, ts]
                            P.add("pe", (lambda e, pb=pb, lhsT=lhsT, rhs=rhs, k=k: e.matmul(pb, lhsT=lhsT, rhs=rhs, start=(k == 0), stop=(k == KC - 1))),
                                  reads=[lhsT, rhs], writes=[pb])
                    self.act_copy(b_, pbs[0])
                    self.act_copy(c_, pbs[1])
                    chd = ch[:, 2 + tb * TB: 2 + (tb + 1) * TB]
                    P.add("dve", (lambda e, chd=chd, ph=pbs[2], c_=c_: e.tensor_tensor(out=chd, in0=ph, in1=c_, op=ALU.mult)),
                          reads=[pbs[2], c_], writes=[chd])
                    s0 = ch[:, tb * TB: (tb + 1) * TB]
                    s1 = ch[:, 1 + tb * TB: 1 + (tb + 1) * TB]
                    if "noz" in dbg:
                        dst = yT[:, fc, ts]
                        P.add("dve", (lambda e, dst=dst, chd=chd, b_=b_: e.tensor_tensor(out=dst, in0=chd, in1=b_, op=ALU.mult)),
                              reads=[chd, b_], writes=[dst])
                        continue
                    P.add("dve", (lambda e, z_=z_, s0=s0, k0=k0: e.tensor_scalar(out=z_, in0=s0, scalar1=k0, scalar2=None, op0=ALU.mult)),
                          reads=[s0, self.PAR], writes=[z_])
                    P.add("dve", (lambda e, z_=z_, s1=s1, k1=k1: e.scalar_tensor_tensor(out=z_, in0=s1, scalar=k1, in1=z_, op0=ALU.mult, op1=ALU.add)),
                          reads=[s1, z_, self.PAR], writes=[z_])
                    P.add("dve", (lambda e, z_=z_, chd=chd, k2=k2: e.scalar_tensor_tensor(out=z_, in0=chd, scalar=k2, in1=z_, op0=ALU.mult, op1=ALU.add)),
                          reads=[chd, z_, self.PAR], writes=[z_])
                    dst = yT[:, fc, ts]
                    P.add("dve", (lambda e, dst=dst, z_=z_, b_=b_: e.tensor_tensor(out=dst, in0=z_, in1=b_, op=ALU.mult)),
                          reads=[z_, b_], writes=[dst])
            for t in (tb_, tc_, th_):
                P.wrelease(t)
        for i in range(2):
            t = self.wtiles_cols(T["conv_w_out"][j], i * 512, 1)[0]
            if P.dry:
                continue
            wv = P.wview(t, KC, 512)
            self.lin_fm(wv, KC, 4, lambda k, tb: yT[:, k, tb * TB:(tb + 1) * TB],
                        lambda pb, mi, tb, i=i: self.add_to_h(pb, i * 4 + mi, tb))
            P.wrelease(t)

    def mm(self, out, lhsT, rhs, start=True, stop=True):
        self.P.add("pe", (lambda e: e.matmul(out, lhsT=lhsT, rhs=rhs, start=start, stop=stop)), reads=[lhsT, rhs], writes=[out])

    def tr(self, out, in_):
        self.P.add("pe", (lambda e: e.transpose(out, in_, self.IDENT)), reads=[in_, self.IDENT], writes=[out])

    def dve(self, fn, reads, writes):
        self.P.add("dve", fn, reads=reads, writes=writes)

    def gmlp(self, l, j):
        P, T = self.P, self.T
        self.rmsnorm(self.cfg["g_mix"] + l * KC)
        self.salloc_reset()
        HALF = 3072
        vb = self.salloc((4, HALF), BF16)
        gtb = [self.salloc((KC, TB), BF16) for _ in range(2)]
        u_sb = [self.salloc((TB,), F32) for _ in range(2)]
        tmp = [self.salloc((4, 128), F32) for _ in range(2)]
        t1b = [self.salloc((128,), F32) for _ in range(2)]
        st = self.salloc((4, 6, 6), F32)
        mv = self.salloc((4, 2), F32)
        sd = self.salloc((4,), F32)
        rstd = self.salloc((4,), F32)
        ws32 = self.salloc((4, 128), F32)
        msk = self.salloc((128,), F32)
        WmT = self.salloc((4, 128), BF16)
        cb = self.salloc((4, 128), F32)
        bsb = self.salloc((4, 128), F32)
        w_in = T["gmlp_w_in"][j]
        w_out = T["gmlp_w_out"][j].rearrange("(kc p) n -> p kc n", p=128)
        cfg = self.cfg
        XN = None if P.dry else self.XN
        if not P.dry:
            P.add("sp", lambda e: e.dma_start(out=ws32, in_=T["gm_wsT"].rearrange("p (g t) -> p g t", g=4)), writes=[ws32], dma="gm")
            P.add("sp", lambda e: e.dma_start(out=msk, in_=T["gm_mask"][:, :]), writes=[msk], dma="gm")
            P.add("sp", lambda e: e.dma_start(out=bsb, in_=T["gm_bsb"].rearrange("p (g t) -> p g t", g=4)), writes=[bsb], dma="gm")
            pbc = P.bank()
            for g in range(4):
                self.dve(lambda e, g=g: e.tensor_tensor(out=WmT[:, g, :], in0=ws32[:, g, :], in1=msk, op=ALU.mult),
                         [ws32[:, g, :], msk], [WmT[:, g, :]])
                self.mm(pbc[:, g * 128:(g + 1) * 128], self.ONES, WmT[:, g, :])
            self.act_copy(cb, pbc[:, 0:512].rearrange("p (g t) -> p g t", g=4))
        it = 0
        for tb in range(NTB):
            for ct in range(6):
                t = self.wtiles_cols(w_in, HALF + ct * 512, 1)[0]
                if P.dry:
                    continue
                wv = P.wview(t, KC, 512)
                for tt in range(4):
                    pb = P.bank()
                    tok = slice(tb * TB + tt * 128, tb * TB + (tt + 1) * 128)
                    for k in range(KC):
                        self.mm(pb, XN[:, k, tok], wv[:, k, :], start=(k == 0), stop=(k == KC - 1))
                    dst = vb[:, tt, ct * 512:(ct + 1) * 512]
                    self.act_copy(dst, pb, func=AF.Gelu)
                    self.dve(lambda e, dst=dst, tt=tt, ct=ct: e.bn_stats(out=st[:, tt, ct, :], in_=dst), [dst], [st[:, tt, ct, :]])
                P.wrelease(t)
            if not P.dry:
                for tt in range(4):
                    sin_ = st[:, tt, :, :].rearrange("p a b -> p (a b)")
                    self.dve(lambda e, tt=tt, sin_=sin_: e.bn_aggr(out=mv[:, tt, :], in_=sin_), [sin_], [mv[:, tt, :]])
                    P.add("act", (lambda e, tt=tt: e.activation(out=sd[:, tt:tt + 1], in_=mv[:, tt, 1:2], func=AF.Sqrt, bias=self.par(cfg["eps_gn"]), scale=1.0)),
                          reads=[mv[:, tt, 1:2], self.PAR], writes=[sd[:, tt:tt + 1]])
                    self.dve(lambda e, tt=tt: e.reciprocal(out=rstd[:, tt:tt + 1], in_=sd[:, tt:tt + 1]), [sd[:, tt:tt + 1]], [rstd[:, tt:tt + 1]])
                    self.dve(lambda e, tt=tt: e.tensor_scalar(out=vb[:, tt, :], in0=vb[:, tt, :], scalar1=mv[:, tt, 0:1], scalar2=rstd[:, tt:tt + 1], op0=ALU.subtract, op1=ALU.mult),
                             [vb[:, tt, :], mv[:, tt, 0:1], rstd[:, tt:tt + 1]], [vb[:, tt, :]])
            for cg in range(3):
                gt = gtb[cg % 2]
                for cc in range(KC):
                    c = cg * KC + cc
                    g = c // 6
                    if c % 4 == 0:
                        tu = self.wtiles_cols(w_in, (c // 4) * 512, 1)[0]
                        wu = None if P.dry else P.wview(tu, KC, 512)
                    if not P.dry:
                        pbu = P.bank()
                        for k in range(KC):
                            self.mm(pbu, wu[:, k, (c % 4) * 128:(c % 4 + 1) * 128], XN[:, k, tb * TB:(tb + 1) * TB], start=(k == 0), stop=(k == KC - 1))
                        us = u_sb[it % 2]
                        tm = tmp[it % 2]
                        t1 = t1b[it % 2]
                        it += 1
                        self.act_copy(us, pbu, func=AF.Gelu)
                        pbs = P.bank()
                        for tt in range(4):
                            self.mm(pbs[:, tt * 128:(tt + 1) * 128], vb[:, tt, c * 128:(c + 1) * 128], WmT[:, g, :])
                        lnb = self.par(cfg["gm_ln_b"] + c)
                        lng = self.par(cfg["gm_ln_g"] + c)
                        self.dve(lambda e, t1=t1, g=g, lnb=lnb: e.scalar_tensor_tensor(out=t1, in0=cb[:, g, :], scalar=lnb, in1=bsb[:, g, :], op0=ALU.mult, op1=ALU.add),
                                 [cb[:, g, :], bsb[:, g, :], self.PAR], [t1])
                        for tt in range(4):
                            self.dve(lambda e, tm=tm, pbs=pbs, t1=t1, lng=lng, tt=tt: e.scalar_tensor_tensor(out=tm[:, tt, :], in0=pbs[:, tt * 128:(tt + 1) * 128], scalar=lng, in1=t1, op0=ALU.mult, op1=ALU.add),
                                     [pbs[:, tt * 128:(tt + 1) * 128], t1, self.PAR], [tm[:, tt, :]])
                        tmf = tm.rearrange("p a b -> p (a b)")
                        self.dve(lambda e, gt=gt, cc=cc, tmf=tmf, us=us: e.tensor_tensor(out=gt[:, cc, :], in0=tmf, in1=us, op=ALU.mult),
                                 [tmf, us], [gt[:, cc, :]])
                    if c % 4 == 3:
                        P.wrelease(tu)
                for i in range(2):
                    t = P.wtile([(w_out[:, cg * KC:(cg + 1) * KC, i * 512:(i + 1) * 512], 0, KC, 512)])
                    if P.dry:
                        continue
                    wv = P.wview(t, KC, 512)
                    self.lin_fm(wv, KC, 4, lambda k, tb_, gt=gt: gt[:, k, :],
                                lambda pb, mi, tb_, i=i: self.add_to_h(pb, i * 4 + mi, tb_), tbs=[tb])
                    P.wrelease(t)

    def retention(self, l, j):
        P, T = self.P, self.T
        cfg = self.cfg
        self.rmsnorm(cfg["g_mix"] + l * KC)
        self.salloc_reset()
        cosT = self.salloc((S,), F32)
        sinT = self.salloc((S,), F32)
        mark = self._so
        A1 = self.salloc((S,), F32)
        A2 = self.salloc((S,), F32)
        A3 = self.salloc((S,), F32)
        PI = math.pi
        if not P.dry:
            posi = A1.bitcast(I32)
            ni = A3.bitcast(I32)
            P.add("sp", lambda e: e.dma_start(out=posi, in_=T["pos_bc"][:, :]), writes=[posi], dma="pos")
            invf = self.par(cfg["inv_freq"])
            C1 = 6.28125
            C2 = 2.0 * PI - 6.28125
            self.dve(lambda e: e.tensor_scalar(out=A2, in0=posi, scalar1=invf, scalar2=None, op0=ALU.mult), [posi, self.PAR], [A2])
            self.dve(lambda e: e.tensor_scalar(out=ni, in0=A2, scalar1=1.0 / (2.0 * PI), scalar2=None, op0=ALU.mult), [A2], [ni])
            self.dve(lambda e: e.scalar_tensor_tensor(out=A2, in0=ni, scalar=-C1, in1=A2, op0=ALU.mult, op1=ALU.add), [ni, A2], [A2])
            self.dve(lambda e: e.scalar_tensor_tensor(out=A2, in0=ni, scalar=-C2, in1=A2, op0=ALU.mult, op1=ALU.add), [ni, A2], [A2])
            for (shift, dstT) in ((0.0, sinT), (PI / 2.0, cosT)):
                self.dve(lambda e, shift=shift: e.tensor_scalar(out=A1, in0=A2, scalar1=shift, scalar2=None, op0=ALU.add), [A2], [A1])
                self.dve(lambda e: e.tensor_scalar(out=A3, in0=A1, scalar1=PI, scalar2=-2.0 * PI, op0=ALU.is_gt, op1=ALU.mult), [A1], [A3])
                self.dve(lambda e: e.tensor_tensor(out=A1, in0=A1, in1=A3, op=ALU.add), [A1, A3], [A1])
                self.dve(lambda e: e.tensor_scalar(out=A3, in0=A1, scalar1=-PI, scalar2=2.0 * PI, op0=ALU.is_lt, op1=ALU.mult), [A1], [A3])
                self.dve(lambda e: e.tensor_tensor(out=A1, in0=A1, in1=A3, op=ALU.add), [A1, A3], [A1])
                self.dve(lambda e: e.tensor_scalar(out=A1, in0=A1, scalar1=-3.141592, scalar2=3.141592, op0=ALU.max, op1=ALU.min), [A1], [A1])
                self.act_copy(dstT, A1, func=AF.Sin)
        self._so = mark
        qTs = [self.salloc((2, TB), BF16) for _ in range(2)]
        kTs = [self.salloc((2, TB), BF16) for _ in range(2)]
        kzs = [self.salloc((4, 256), BF16) for _ in range(2)]
        v_sbs = [self.salloc((4, TB), BF16) for _ in range(2)]
        g_sbs = [self.salloc((4, TB), BF16) for _ in range(2)]
        goT = self.salloc((4, TB), BF16)
        rt = [self.salloc((TB,), F32) for _ in range(2)]
        xs = [self.salloc((TB,), F32) for _ in range(2)]
        sTs = [self.salloc((128,), BF16) for _ in range(2)]
        R32 = self.salloc((2, TB), F32)
        Rbf = [self.salloc((2, TB), BF16) for _ in range(2)]
        t1 = rt[0]
        gos = [self.salloc((TB,), BF16) for _ in range(2)]
        st6 = [self.salloc((6,), F32) for _ in range(2)]
        mvs = [self.salloc((2,), F32) for _ in range(2)]
        sds = [self.salloc((1,), F32) for _ in range(2)]
        rss = [self.salloc((1,), F32) for _ in range(2)]
        maskT = self.salloc((4, 128), F32)
        w_in = T["ret_w_in"][j].rearrange("(kc p) n -> p kc n", p=128)
        w_out = T["ret_w_out"][j].rearrange("(ec p) n -> p ec n", p=128)

        def req_head(h):
            tqk = P.wtile([(w_in[:, :, h * 256:(h + 1) * 256], 0, KC, 256),
                           (w_in[:, :, D + h * 256: D + (h + 1) * 256], KC * 256, KC, 256)])
            tv = P.wtile([(w_in[:, :, 2 * D + h * 512: 2 * D + (h + 1) * 512], 0, KC, 512)])
            tg = P.wtile([(w_in[:, :, 4 * D + h * 512: 4 * D + (h + 1) * 512], 0, KC, 512)])
            two = P.wtile([(w_out[:, h * 4:(h + 1) * 4, :], 0, 4, D)])
            if P.dry:
                return None
            return dict(tqk=tqk, tv=tv, tg=tg, two=two,
                        wq=P.wview(tqk, KC, 256), wk=P.wview(tqk, KC, 256, eoff=KC * 256),
                        wv=P.wview(tv, KC, 512), wg=P.wview(tg, KC, 512), wo=P.wview(two, 4, D))

        if P.dry:
            for h in range(RET_HEADS):
                req_head(h)
            return
        XN = self.XN
        P.add("sp", lambda e: e.dma_start(out=maskT, in_=T["ret_mask"].rearrange("p (h t) -> p h t", h=4)), writes=[maskT], dma="rmask")
        P.pools = {"proj": [0, 1, 2], "S": [3], "o": [4, 5], "r": [6, 7], "t": [3]}
        units = [(h, tb) for h in range(RET_HEADS) for tb in range(NTB)]
        HW = {}

        def proj_pieces(ui):
            h, tb = units[ui]
            bs = ui % 2
            qT, kT, v_sb, g_sb = qTs[bs], kTs[bs], v_sbs[bs], g_sbs[bs]
            ts = slice(tb * TB, (tb + 1) * TB)
            cs = cosT[:, ts]
            sn = sinT[:, ts]

            def qk(which):
                if which == 0 and tb == 0:
                    HW[h] = req_head(h)
                W = HW[h]
                wx, XT = ((W["wq"], qT), (W["wk"], kT))[which]
                x1, x2 = (xs[0], xs[1]) if which == 0 else (xs[1], xs[0])
                p1 = P.bank(pool="proj")
                for k in range(KC):
                    self.mm(p1, wx[:, k, 0:128], XN[:, k, ts], start=(k == 0), stop=(k == KC - 1))
                self.act_copy(x1, p1)
                p2 = P.bank(pool="proj")
                for k in range(KC):
                    self.mm(p2, wx[:, k, 128:256], XN[:, k, ts], start=(k == 0), stop=(k == KC - 1))
                self.act_copy(x2, p2)
                a, b = rt
                self.dve(lambda e: e.tensor_tensor(out=a, in0=x1, in1=cs, op=ALU.mult), [x1, cs], [a])
                self.dve(lambda e: e.tensor_tensor(out=b, in0=x2, in1=sn, op=ALU.mult), [x2, sn], [b])
                self.dve(lambda e: e.tensor_tensor(out=XT[:, 0, :], in0=a, in1=b, op=ALU.subtract), [a, b], [XT[:, 0, :]])
                self.dve(lambda e: e.tensor_tensor(out=a, in0=x2, in1=cs, op=ALU.mult), [x2, cs], [a])
                self.dve(lambda e: e.tensor_tensor(out=b, in0=x1, in1=sn, op=ALU.mult), [x1, sn], [b])
                self.dve(lambda e: e.tensor_tensor(out=XT[:, 1, :], in0=a, in1=b, op=ALU.add), [a, b], [XT[:, 1, :]])

            def vg(c4):
                W = HW[h]
                tok = slice(tb * TB + c4 * 128, tb * TB + (c4 + 1) * 128)
                pv = P.bank(pool="proj")
                for k in range(KC):
                    self.mm(pv, XN[:, k, tok], W["wv"][:, k, :], start=(k == 0), stop=(k == KC - 1))
                self.act_copy(v_sb[:, c4, :], pv)
                pg = P.bank(pool="proj")
                for k in range(KC):
                    self.mm(pg, XN[:, k, tok], W["wg"][:, k, :], start=(k == 0), stop=(k == KC - 1))
                self.act_copy(g_sb[:, c4, :], pg, func=AF.Silu)
                if c4 == 3 and tb == NTB - 1:
                    for t in (W["tqk"], W["tv"], W["tg"]):
                        P.wrelease(t)

            return [lambda: qk(0), lambda: qk(1), lambda: vg(0), lambda: vg(1), lambda: vg(2), lambda: vg(3), lambda: None]

        def wout_pieces(ui):
            h, tb = units[ui]

            def grp(m0, m1):
                wo = HW[h]["wo"]
                for mi in range(m0, m1):
                    pb = P.bank(pool="proj")
                    for k in range(4):
                        self.mm(pb, wo[:, k, mi * 128:(mi + 1) * 128], goT[:, k, :], start=(k == 0), stop=(k == 3))
                    self.add_to_h(pb, mi, tb)
                if m1 == KC and tb == NTB - 1:
                    P.wrelease(HW[h]["two"])
            return [lambda: grp(0, 3), lambda: grp(3, 6), lambda: grp(6, 8)]

        def unit_steps(ui):
            h, tb = units[ui]
            bs = ui % 2
            qT, kT, kz, v_sb, g_sb = qTs[bs], kTs[bs], kzs[bs], v_sbs[bs], g_sbs[bs]
            gam = 1.0 - 2.0 ** (-5.0 - h)
            decay = gam ** 128
            zcol = self.par(cfg["ret_zeta"] + h)
            ecol = self.par(cfg["ret_epsxi"] + h)
            pos = {}

            def pre():
                if tb == 0:
                    hsrc = self.HT[:, 0, 0:2 * TB].rearrange("p (a b) -> p a b", a=2)
                    self.dve(lambda e: e.tensor_scalar(out=R32, in0=hsrc, scalar1=0.0, scalar2=None, op0=ALU.mult), [hsrc], [R32])
                pkt = P.bank(BF16, pool="t")
                for c4 in range(4):
                    for dc in range(2):
                        self.tr(pkt[:, c4 * 256 + dc * 128: c4 * 256 + (dc + 1) * 128], kT[:, dc, c4 * 128:(c4 + 1) * 128])
                kzf = kz.rearrange("p a b -> p (a b)")
                P.add("act", (lambda e: e.activation(out=kzf, in_=pkt, func=AF.Identity, scale=zcol)),
                      reads=[pkt, self.PAR], writes=[kzf])

            def stA(c4):
                ci = tb * 4 + c4
                cols = slice(c4 * 128, (c4 + 1) * 128)
                pS = P.bank(pool="S")
                pSs = pS[:, 0:128]
                for dc in range(2):
                    self.mm(pSs, kT[:, dc, cols], qT[:, dc, cols], start=(dc == 0), stop=(dc == 1))
                sT = sTs[ci % 2]
                self.dve(lambda e: e.tensor_tensor(out=sT, in0=pSs, in1=maskT[:, h, :], op=ALU.mult), [pSs, maskT[:, h, :]], [sT])
                if ci < 15:
                    Rn = Rbf[ci % 2]
                    for dc in range(2):
                        pr = P.bank(pool="r")
                        self.mm(pr, kz[:, c4, dc * 128:(dc + 1) * 128], v_sb[:, c4, :])
                        self.dve(lambda e, dc=dc, pr=pr: e.scalar_tensor_tensor(out=R32[:, dc, :], in0=R32[:, dc, :], scalar=decay, in1=pr, op0=ALU.mult, op1=ALU.add),
                                 [R32[:, dc, :], pr], [R32[:, dc, :]])
                        self.act_copy(Rn[:, dc, :], R32[:, dc, :])

            def stB(c4):
                ci = tb * 4 + c4
                cols = slice(c4 * 128, (c4 + 1) * 128)
                sT = sTs[ci % 2]
                po = P.bank(pool="o")
                pos[c4] = po
                self.mm(po, sT, v_sb[:, c4, :], start=True, stop=(ci == 0))
                if ci > 0:
                    Rb = Rbf[(ci - 1) % 2]
                    for dc in range(2):
                        self.mm(po, qT[:, dc, cols], Rb[:, dc, :], start=False, stop=(dc == 1))
                s6, mv_, sd_ = st6[ci % 2], mvs[ci % 2], sds[ci % 2]
                self.dve(lambda e: e.bn_stats(out=s6, in_=po), [po], [s6])
                self.dve(lambda e: e.bn_aggr(out=mv_, in_=s6), [s6], [mv_])
                P.add("act", (lambda e: e.activation(out=sd_, in_=mv_[:, 1:2], func=AF.Sqrt, bias=ecol, scale=1.0)),
                      reads=[mv_[:, 1:2], self.PAR], writes=[sd_])

            def stC(c4):
                ci = tb * 4 + c4
                po = pos[c4]
                mv_, sd_, rs_ = mvs[ci % 2], sds[ci % 2], rss[ci % 2]
                go = gos[ci % 2]
                self.dve(lambda e: e.reciprocal(out=rs_, in_=sd_), [sd_], [rs_])
                self.dve(lambda e: e.tensor_scalar(out=t1, in0=po, scalar1=mv_[:, 0:1], scalar2=rs_, op0=ALU.subtract, op1=ALU.mult),
                         [po, mv_[:, 0:1], rs_], [t1])
                self.dve(lambda e: e.tensor_tensor(out=go, in0=t1, in1=g_sb[:, c4, :], op=ALU.mult), [t1, g_sb[:, c4, :]], [go])

            def stD(c4):
                ci = tb * 4 + c4
                go = gos[ci % 2]
                pgt = P.bank(BF16, pool="t")
                for ec in range(4):
                    self.tr(pgt[:, ec * 128:(ec + 1) * 128], go[:, ec * 128:(ec + 1) * 128])
                for ec in range(4):
                    gcol = self.par(cfg["ret_gn_g"] + j * 16 + h * 4 + ec)
                    dst = goT[:, ec, c4 * 128:(c4 + 1) * 128]
                    src = pgt[:, ec * 128:(ec + 1) * 128]
                    P.add("act", (lambda e, dst=dst, src=src, gcol=gcol: e.activation(out=dst, in_=src, func=AF.Identity, scale=gcol)),
                          reads=[src, self.PAR], writes=[dst])

            def step(sidx):
                if sidx == 0:
                    pre()
                if 0 <= sidx - 3 < 4:
                    stD(sidx - 3)
                if 0 <= sidx - 2 < 4:
                    stC(sidx - 2)
                if 0 <= sidx - 1 < 4:
                    stB(sidx - 1)
                if sidx < 4:
                    stA(sidx)
            return step

        for pc in proj_pieces(0):
            pc()
        for ui in range(len(units)):
            nxt = proj_pieces(ui + 1) if ui + 1 < len(units) else [lambda: None] * 7
            prevw = wout_pieces(ui - 1) if ui > 0 else []
            step = unit_steps(ui)
            for sidx in range(7):
                step(sidx)
                if sidx < len(prevw):
                    prevw[sidx]()
                nxt[sidx]()
        for pc in wout_pieces(len(units) - 1):
            pc()
        P.pools = {}

    def epilogue(self):
        P, T = self.P, self.T
        self.rmsnorm(self.cfg["g_final"], inplace=True)
        if P.dry:
            return
        oT = T["outT"].rearrange("(c p) t -> p c t", p=128)
        for c in range(KC):
            src = self.HT[:, c, :]
            P.add("sp", (lambda e, src=src, c=c: e.dma_start(out=oT[:, c, :], in_=src)), reads=[src], dma="out")


def make_params(inputs, cfg_only=False):
    cols = []
    cfg = {}

    def addvec(name, v):
        n = v.shape[0] // 128
        cfg_name_start = sum(c.shape[1] for c in cols)
        cols.append(np.ascontiguousarray(v.reshape(n, 128).T))
        return cfg_name_start

    def addconst(val):
        st = sum(c.shape[1] for c in cols)
        cols.append(np.full((128, 1), val, np.float32))
        return st

    cfg["g_mix"] = addvec("g_mix", inputs["norm_mix_g"].reshape(-1))
    cfg["g_xa"] = addvec("g_xa", inputs["norm_xa_g"].reshape(-1))
    cfg["g_mem"] = addvec("g_mem", inputs["norm_mem_g"].reshape(-1))
    cfg["g_ffn"] = addvec("g_ffn", inputs["norm_ffn_g"].reshape(-1))
    cfg["g_final"] = addvec("g_final", inputs["norm_f_g"].reshape(-1))
    cfg["eps_norm"] = addconst(NORM_EPS)
    cfg["conv_k"] = addvec("conv_k", inputs["conv_k"][0].reshape(-1))
    cfg["eps_gn"] = addconst(GN_EPS)
    cfg["gm_ln_g"] = addvec("gm_ln_g", inputs["gmlp_ln_g"][0])
    cfg["gm_ln_b"] = addvec("gm_ln_b", inputs["gmlp_ln_b"][0])
    cfg["ret_gn_g"] = addvec("ret_gn_g", inputs["ret_gn_g"].reshape(-1))
    half = 128
    inv = (10000.0 ** (-np.arange(half, dtype=np.float32) / half)).astype(np.float32)
    cfg["inv_freq"] = addvec("inv_freq", inv)
    idx = np.arange(128, dtype=np.float64)
    zs, es = [], []
    for h in range(RET_HEADS):
        gam = 1.0 - 2.0 ** (-5.0 - h)
        zs.append((gam ** (127.0 - idx)) / 16.0)
        es.append(GN_EPS / (gam ** (2.0 * (idx + 1.0))))
    cfg["ret_zeta"] = sum(c.shape[1] for c in cols)
    cols.append(np.stack(zs, 1).astype(np.float32))
    cfg["ret_epsxi"] = sum(c.shape[1] for c in cols)
    cols.append(np.stack(es, 1).astype(np.float32))
    par = np.concatenate(cols, axis=1).astype(np.float32)
    cfg["npar"] = par.shape[1]
    return par, cfg


def ret_mask_const():
    sidx = np.arange(128, dtype=np.float64)[:, None]
    tidx = np.arange(128, dtype=np.float64)[None, :]
    out = np.zeros((128, RET_HEADS, 128), np.float32)
    for h in range(RET_HEADS):
        gam = 1.0 - 2.0 ** (-5.0 - h)
        out[:, h, :] = np.where(tidx >= sidx, gam ** (-sidx - 1.0) / 16.0, 0.0)
    return np.ascontiguousarray(out.reshape(128, RET_HEADS * 128))


def build_program(cfg, stages):
    nc = bass.Bass("TRN2", target_bir_lowering=False)
    T = {}

    def din(name, shape, dt=F32):
        T[name] = nc.dram_tensor(name, list(shape), dt, kind="ExternalInput").ap()

    din("xT", (D, S))
    din("params", (128, cfg["npar"]))
    din("ones_bf", (128, 128), BF16)
    din("ident_bf", (128, 128), BF16)
    din("ffn_w1", (DEPTH, D, 4 * D))
    din("ffn_w2", (DEPTH, 4 * D, D))
    din("memT", (D, MEM))
    din("xa_w_q", (DEPTH, D, D))
    din("xa_w_kv", (DEPTH, D, 2 * D))
    din("xa_w_o", (DEPTH, D, D))
    din("conv_w_in", (1, D, 3 * D))
    din("conv_w_out", (1, D, D))
    din("gmlp_w_in", (1, D, 6 * D))
    din("gmlp_w_out", (1, 3 * D, D))
    din("gm_wsT", (128, 512))
    din("gm_mask", (128, 128))
    din("gm_bsb", (128, 512))
    din("ret_w_in", (2, D, 6 * D))
    din("ret_w_out", (2, 2 * D, D))
    din("ret_mask", (128, 512))
    din("pos_bc", (128, S), I32)
    T["outT"] = nc.dram_tensor("outT", [D, S], F32, kind="ExternalOutput").ap()

    def program(P):
        B = Builder(P, T, cfg)
        B.prologue()
        stages(B)
        B.epilogue()

    Pd = Prog(nc, None, None, dry=True)
    program(Pd)
    sched = Pd.sched
    import contextlib
    with contextlib.ExitStack() as st:
        sb = st.enter_context(nc.sbuf_tensor("sb", [128, 212480], U8))
        ps = st.enter_context(nc.psum_tensor("ps", [128, 8, 2048], U8))
        P = Prog(nc, sb, ps, dry=False, sched=sched)
        program(P)
        P.emit()
    return nc


def default_stages(B):
    for l in range(DEPTH):
        kind, j = l % 3, l // 3
        if kind == 0:
            B.retention(l, j)
        elif kind == 1:
            B.gmlp(l, j)
        else:
            B.conv(l, j)
        B.xattn(l)
        B.ffn(l)


def host_inputs(inputs, b, par):
    import ml_dtypes
    m = {
        "xT": np.ascontiguousarray(inputs["x"][b].T),
        "params": par,
        "ones_bf": np.ones((128, 128), ml_dtypes.bfloat16),
        "ident_bf": np.eye(128, dtype=np.float32).astype(ml_dtypes.bfloat16),
        "ffn_w1": inputs["ffn_w1"],
        "ffn_w2": inputs["ffn_w2"],
        "memT": np.ascontiguousarray(inputs["mem"][b].T),
        "xa_w_q": inputs["xa_w_q"], "xa_w_kv": inputs["xa_w_kv"], "xa_w_o": inputs["xa_w_o"],
        "conv_w_in": inputs["conv_w_in"], "conv_w_out": inputs["conv_w_out"],
        "gmlp_w_in": inputs["gmlp_w_in"], "gmlp_w_out": inputs["gmlp_w_out"],
        "gm_wsT": np.ascontiguousarray(inputs["gmlp_w_s"][0].transpose(2, 0, 1).reshape(128, 512)),
        "gm_mask": np.triu(np.ones((128, 128), np.float32)),
        "gm_bsb": np.ascontiguousarray(np.broadcast_to(inputs["gmlp_b_s"][0].reshape(1, 512), (128, 512))),
        "ret_w_in": inputs["ret_w_in"], "ret_w_out": inputs["ret_w_out"],
        "ret_mask": ret_mask_const(),
        "pos_bc": np.ascontiguousarray(np.broadcast_to(inputs["positions"][b][None, :], (128, S))).astype(np.int32),
    }
    return m


def kernel(**inputs):
    inputs = {k: np.asarray(v) for k, v in inputs.items()}
    par, cfg = make_params(inputs)
    nc = build_program(cfg, default_stages)
    in_maps = [host_inputs(inputs, b, par) for b in range(8)]
    res = run_bass_kernel_spmd(nc, in_maps, core_ids=list(range(8)))
    out = np.stack([np.ascontiguousarray(r["outT"].T) for r in res.results], axis=0)
    return out.astype(np.float32)
```

```python
import math
import numpy as np
import concourse.bass as bass
import concourse.mybir as mybir
from concourse.bass_utils import run_bass_kernel_spmd

F32 = mybir.dt.float32
BF16 = mybir.dt.bfloat16
I32 = mybir.dt.int32
U8 = mybir.dt.uint8
AF = mybir.ActivationFunctionType
ALU = mybir.AluOpType

D = 1024
S = 2048
DEPTH = 4
KC = 8
NTB = 4
TB = 512
MEM = 256
NSLOT = 5
SLOT_BYTES = 8192
RET_HEADS = 4
NORM_EPS = 1e-6
GN_EPS = 1e-5

_ESZ = {F32: 4, BF16: 2, I32: 4, U8: 1}


def _prod(xs):
    r = 1
    for x in xs:
        r *= int(x)
    return r


class Op:
    __slots__ = ("eng", "fn", "deps", "waits", "inc", "dma", "idx", "dmaval")

    def __init__(self, eng, fn, dma):
        self.eng = eng
        self.fn = fn
        self.deps = []
        self.waits = []
        self.inc = False
        self.dma = dma
        self.idx = -1
        self.dmaval = 0


class Prog:
    ENGS = ("pe", "act", "dve", "pool", "sp")

    def __init__(self, nc, sb, ps, dry=False, sched=None):
        self.nc = nc
        self.sb = sb
        self.ps = ps
        self.dry = dry
        self.ops = {e: [] for e in self.ENGS}
        self.rec = {}
        self.wm = {e: {} for e in self.ENGS}
        self.dmacount = {}
        self.nbank = 0
        self.pools = {}
        self.sched = sched if sched is not None else []
        self.wreq = 0
        self.wissued = 0
        self.free_slots = list(range(NSLOT))
        self.tile_slot = {}
        self.sb_off = 0

    def sbv(self, off, shape, dt):
        n = _prod(shape) * _ESZ[dt]
        v = self.sb[:, off:off + n].bitcast(dt)
        if len(shape) == 2:
            v = v.rearrange("p (a b) -> p a b", a=shape[0])
        elif len(shape) == 3:
            v = v.rearrange("p (a b c) -> p a b c", a=shape[0], b=shape[1])
        return v

    def bank(self, dt=F32, pool=None):
        if pool is not None and pool in self.pools:
            lst = self.pools[pool]
            b = lst[0]
            lst.append(lst.pop(0))
        else:
            b = self.nbank
            self.nbank = (self.nbank + 1) % 8
        return self.ps[:, b, :].bitcast(dt)

    @staticmethod
    def region(ap):
        t = ap.tensor
        esz = _ESZ[ap.dtype]
        row = _prod(t.shape[1:])
        off = int(ap.offset)
        p0 = off // row
        f0 = off % row
        a = ap.ap
        npart = a[0][1]
        lo = f0
        hi = f0
        for step, cnt in a[1:]:
            if step >= 0:
                hi += step * (cnt - 1)
            else:
                lo += step * (cnt - 1)
        blo, bhi = lo * esz, (hi + 1) * esz
        if t.name == "ps":
            blo = (blo // 2048) * 2048
            bhi = ((bhi + 2047) // 2048) * 2048
        return t.name, p0, p0 + npart, blo, bhi

    def add(self, eng, fn, reads=(), writes=(), dma=None):
        if self.dry:
            return
        op = Op(eng, fn, dma)
        lst = self.ops[eng]
        op.idx = len(lst)
        lst.append(op)
        if dma is not None:
            self.dmacount.setdefault(dma, 0)
            op.dmaval = self.dmacount[dma] + 16
            tok = ("dma", dma)
        else:
            tok = ("eng", eng)
        deps = {}

        myval = op.dmaval if dma is not None else op.idx

        def need(t, v):
            if t == tok and v == myval:
                return
            if t[0] == "dma":
                v = self.dmacount[t[1]]
            if deps.get(t, -1) < v:
                deps[t] = v

        for ap in reads:
            name, p0, p1, lo, hi = self.region(ap)
            for r in self.rec.get(name, ()):
                if r[0] < p1 and p0 < r[1] and r[2] < hi and lo < r[3]:
                    if r[4] is not None:
                        need(*r[4])
                    r[5][tok] = myval
        for ap in writes:
            name, p0, p1, lo, hi = self.region(ap)
            lst2 = self.rec.setdefault(name, [])
            keep = []
            for r in lst2:
                if r[0] < p1 and p0 < r[1] and r[2] < hi and lo < r[3]:
                    if r[4] is not None:
                        need(*r[4])
                    for t, v in r[5].items():
                        need(t, v)
                    if p0 <= r[0] and r[1] <= p1 and lo <= r[2] and r[3] <= hi:
                        continue
                keep.append(r)
            keep.append([p0, p1, lo, hi, (tok, myval), {}])
            self.rec[name] = keep
        if dma is not None:
            self.dmacount[dma] = op.dmaval
        wm = self.wm[eng]
        for t, v in deps.items():
            if t == tok and dma is None and eng == "pe":
                continue
            if t == tok and dma is None and v >= op.idx:
                continue
            if wm.get(t, -1) >= v:
                continue
            wm[t] = v
            op.waits.append((t, v))
            if t[0] == "eng":
                self.ops[t[1]][v].inc = True

    def wtile(self, pieces):
        i = self.wreq
        self.wreq += 1
        if self.dry:
            self.sched.append(pieces)
            return 0
        self._issue()
        if i not in self.tile_slot:
            raise RuntimeError("weight ring too small at tile %d" % i)
        return i

    def wview(self, i, a, b, eoff=0):
        if self.dry:
            return None
        s = self.tile_slot[i]
        return self.sbv(self.WR_OFF + s * SLOT_BYTES + eoff * 2, (a, b), BF16)

    def wrelease(self, i):
        if self.dry:
            return
        self.free_slots.append(self.tile_slot[i])
        self._issue()

    def _issue(self):
        while self.free_slots and self.wissued < len(self.sched):
            s = self.free_slots.pop(0)
            t = self.wissued
            self.wissued += 1
            self.tile_slot[t] = s
            for (src, eoff, a, b) in self.sched[t]:
                dst = self.sbv(self.WR_OFF + s * SLOT_BYTES + eoff * 2, (a, b), BF16)
                self.add("pool", (lambda e, dst=dst, src=src: e.dma_start(out=dst, in_=src)),
                         writes=[dst], dma="w%d" % s)

    def emit(self, final_dma_keys=("out",)):
        nc = self.nc
        import contextlib
        with contextlib.ExitStack() as st:
            esem = {e: st.enter_context(nc.semaphore("s_" + e)) for e in self.ENGS}
            dsem = {k: st.enter_context(nc.semaphore("d_" + k)) for k in self.dmacount}
            incval = {}
            for e in self.ENGS:
                c = 0
                vals = []
                for op in self.ops[e]:
                    if op.inc:
                        c += 1
                    vals.append(c)
                incval[e] = vals
            block = st.enter_context(nc.Block())

            def run(engname, eng):
                for op in self.ops[engname]:
                    for (t, v) in op.waits:
                        if t[0] == "eng":
                            eng.wait_ge(esem[t[1]], incval[t[1]][v])
                        else:
                            eng.wait_ge(dsem[t[1]], v)
                    ins = op.fn(eng)
                    if op.dma is not None:
                        ins.then_inc(dsem[op.dma], 16)
                    elif op.inc:
                        ins.then_inc(esem[engname], 1)
                if engname == "sp":
                    for k in final_dma_keys:
                        eng.wait_ge(dsem[k], self.dmacount[k])

            block.tensor(lambda e: run("pe", e))
            block.scalar(lambda e: run("act", e))
            block.vector(lambda e: run("dve", e))
            block.gpsimd(lambda e: run("pool", e))
            block.sync(lambda e: run("sp", e))


class Builder:
    def __init__(self, P, T, cfg):
        self.P = P
        self.T = T
        self.cfg = cfg
        P.WR_OFF = 0
        o = NSLOT * SLOT_BYTES
        self.HT = P.sbv(o, (KC, S), F32) if not P.dry else None
        self.HT_OFF = o
        o += KC * S * 4
        self.XN_OFF = o
        o += KC * S * 2
        self.PAR_OFF = o
        o += cfg["npar"] * 4
        self.ONES_OFF = o
        o += 256
        self.ID_OFF = o
        o += 256
        self.ZC_OFF = o
        o += 64
        self.SCR = o
        self.SCR_END = 212000
        if not P.dry:
            self.XN = P.sbv(self.XN_OFF, (KC, S), BF16)
            self.PAR = P.sbv(self.PAR_OFF, (cfg["npar"],), F32)
            self.ONES = P.sbv(self.ONES_OFF, (128,), BF16)
            self.IDENT = P.sbv(self.ID_OFF, (128,), BF16)

    def salloc_reset(self):
        self._so = self.SCR

    def salloc(self, shape, dt):
        n = _prod(shape) * _ESZ[dt]
        n = (n + 63) // 64 * 64
        off = self._so
        self._so += n
        assert self._so <= self.SCR_END, ("scratch overflow", self._so)
        if self.P.dry:
            return None
        return self.P.sbv(off, shape, dt)

    def par(self, col, n=1):
        return self.PAR[:, col:col + n]

    def prologue(self):
        P, T = self.P, self.T
        if P.dry:
            return
        xT = T["xT"].rearrange("(c p) t -> p c t", p=128)
        for c in range(KC):
            dst = self.HT[:, c, :]
            P.add("sp", (lambda e, dst=dst, c=c: e.dma_start(out=dst, in_=xT[:, c, :])), writes=[dst], dma="in")
        P.add("sp", lambda e: e.dma_start(out=self.PAR, in_=T["params"][:, :]), writes=[self.PAR], dma="in")
        P.add("sp", lambda e: e.dma_start(out=self.ONES, in_=T["ones_bf"][:, :]), writes=[self.ONES], dma="in")
        P.add("sp", lambda e: e.dma_start(out=self.IDENT, in_=T["ident_bf"][:, :]), writes=[self.IDENT], dma="in")

    def rmsnorm(self, gcol, inplace=False):
        P = self.P
        self.salloc_reset()
        sq = self.salloc((KC, TB), BF16)
        rs = [self.salloc((TB,), F32) for _ in range(2)]
        rr = [self.salloc((TB,), F32) for _ in range(2)]
        if P.dry:
            return
        for tb in range(NTB):
            ts = slice(tb * TB, (tb + 1) * TB)
            pb = P.bank()
            for c in range(KC):
                src = self.HT[:, c, ts]
                dst = sq[:, c, :]
                P.add("act", (lambda e, dst=dst, src=src: e.activation(out=dst, in_=src, func=AF.Square)),
                      reads=[src], writes=[dst])
                P.add("pe", (lambda e, c=c, dst=dst, pb=pb: e.matmul(pb, lhsT=self.ONES, rhs=dst, start=(c == 0), stop=(c == KC - 1))),
                      reads=[self.ONES, dst], writes=[pb])
            r1 = rs[tb % 2]
            r2 = rr[tb % 2]
            P.add("act", (lambda e, r1=r1, pb=pb: e.activation(out=r1, in_=pb, func=AF.Ln, scale=1.0 / D, bias=self.par(self.cfg["eps_norm"]))),
                  reads=[pb, self.PAR], writes=[r1])
            P.add("act", (lambda e, r1=r1, r2=r2: e.activation(out=r2, in_=r1, func=AF.Exp, scale=-0.5)), reads=[r1], writes=[r2])
            for c in range(KC):
                src = self.HT[:, c, ts]
                dst = src if inplace else self.XN[:, c, ts]
                g = self.par(gcol + c)
                P.add("dve", (lambda e, dst=dst, src=src, g=g, r2=r2: e.scalar_tensor_tensor(out=dst, in0=src, scalar=g, in1=r2, op0=ALU.mult, op1=ALU.mult)),
                      reads=[src, r2, self.PAR], writes=[dst])

    def lin_fm(self, wv, nk, nm, rhs_fn, evac_fn, tbs=range(NTB)):
        P = self.P
        for mi in range(nm):
            for tb in tbs:
                pb = P.bank()
                for k in range(nk):
                    lhsT = wv[:, k, mi * 128:(mi + 1) * 128]
                    rhs = rhs_fn(k, tb)
                    P.add("pe", (lambda e, pb=pb, lhsT=lhsT, rhs=rhs, k=k: e.matmul(pb, lhsT=lhsT, rhs=rhs, start=(k == 0), stop=(k == nk - 1))),
                          reads=[lhsT, rhs], writes=[pb])
                evac_fn(pb, mi, tb)

    def add_to_h(self, pb, m, tb):
        P = self.P
        dst = self.HT[:, m, tb * TB:(tb + 1) * TB]
        P.add("dve", (lambda e, dst=dst, pb=pb: e.tensor_tensor(out=dst, in0=pb, in1=dst, op=ALU.add)),
              reads=[pb, dst], writes=[dst])

    def ffn(self, l):
        P, T = self.P, self.T
        self.rmsnorm(self.cfg["g_ffn"] + l * KC)
        self.salloc_reset()
        h1 = [self.salloc((4, S), BF16) for _ in range(2)]
        r32 = [self.salloc((TB,), F32) for _ in range(3)]
        w1 = T["ffn_w1"][l].rearrange("(kc p) f -> p kc f", p=128)
        w2 = T["ffn_w2"][l].rearrange("(fc p) m -> p fc m", p=128)
        NF = 8
        cnt = [0]

        def w1_phase(j):
            t = P.wtile([(w1[:, :, j * 512:(j + 1) * 512], 0, KC, 512)])
            if P.dry:
                return
            wv = P.wview(t, KC, 512)
            hb = h1[j % 2]

            def evac(pb, mi, tb):
                r = r32[cnt[0] % 3]
                cnt[0] += 1
                dst = hb[:, mi, tb * TB:(tb + 1) * TB]
                P.add("act", (lambda e, r=r, pb=pb: e.activation(out=r, in_=pb, func=AF.Relu)), reads=[pb], writes=[r])
                P.add("act", (lambda e, r=r, dst=dst: e.activation(out=dst, in_=r, func=AF.Square)), reads=[r], writes=[dst])
            self.lin_fm(wv, KC, 4, lambda k, tb: self.XN[:, k, tb * TB:(tb + 1) * TB], evac)
            P.wrelease(t)

        def w2_phase(j):
            t = P.wtile([(w2[:, j * 4:(j + 1) * 4, :], 0, 4, D)])
            if P.dry:
                return
            wv = P.wview(t, 4, D)
            hb = h1[j % 2]
            self.lin_fm(wv, 4, KC, lambda k, tb: hb[:, k, tb * TB:(tb + 1) * TB], self.add_to_h)
            P.wrelease(t)

        w1_phase(0)
        for j in range(NF):
            if j + 1 < NF:
                w1_phase(j + 1)
            w2_phase(j)

    def act_copy(self, dst, src, scale=None, func=None):
        P = self.P
        f = func if func is not None else AF.Copy
        if scale is None:
            P.add("act", (lambda e, dst=dst, src=src, f=f: e.activation(out=dst, in_=src, func=f)), reads=[src], writes=[dst])
        else:
            P.add("act", (lambda e, dst=dst, src=src, f=f, scale=scale: e.activation(out=dst, in_=src, func=f, scale=scale)), reads=[src], writes=[dst])

    def wtiles_cols(self, w, col0, ntiles, width=512):
        P = self.P
        wv = w.rearrange("(kc p) n -> p kc n", p=128)
        return [P.wtile([(wv[:, :, col0 + i * width: col0 + (i + 1) * width], 0, KC, width)]) for i in range(ntiles)]

    def xattn(self, l):
        P, T = self.P, self.T
        self.rmsnorm(self.cfg["g_xa"] + l * KC)
        self.salloc_reset()
        qT = self.salloc((KC, S), BF16)
        memT = self.salloc((KC, MEM), F32)
        msq = self.salloc((KC, MEM), BF16)
        mrs = self.salloc((MEM,), F32)
        mrr = self.salloc((MEM,), F32)
        memn = self.salloc((KC, MEM), BF16)
        kT = self.salloc((KC, MEM), BF16)
        vtok = self.salloc((2, D), BF16)
        pT = [self.salloc((2, TB), BF16) for _ in range(2)]
        rden = [self.salloc((TB,), F32) for _ in range(2)]
        wkv = T["xa_w_kv"][l]
        if P.dry:
            self.wtiles_cols(wkv, 0, 2)
            self.wtiles_cols(wkv, D, 2)
            self.wtiles_cols(T["xa_w_q"][l], 0, 2)
            self.wtiles_cols(T["xa_w_o"][l], 0, 2)
            return
        XN = self.XN
        mT = T["memT"].rearrange("(c p) t -> p c t", p=128)
        P.add("sp", lambda e: e.dma_start(out=memT, in_=mT), writes=[memT], dma="mem")
        pb = P.bank()
        pbm = pb[:, 0:MEM]
        for c in range(KC):
            self.act_copy(msq[:, c, :], memT[:, c, :], func=AF.Square)
            P.add("pe", (lambda e, c=c: e.matmul(pbm, lhsT=self.ONES, rhs=msq[:, c, :], start=(c == 0), stop=(c == KC - 1))),
                  reads=[self.ONES, msq[:, c, :]], writes=[pbm])
        P.add("act", lambda e: e.activation(out=mrs, in_=pbm, func=AF.Ln, scale=1.0 / D, bias=self.par(self.cfg["eps_norm"])),
              reads=[pbm, self.PAR], writes=[mrs])
        P.add("act", lambda e: e.activation(out=mrr, in_=mrs, func=AF.Exp, scale=-0.5), reads=[mrs], writes=[mrr])
        for c in range(KC):
            g = self.par(self.cfg["g_mem"] + l * KC + c)
            P.add("dve", (lambda e, c=c, g=g: e.scalar_tensor_tensor(out=memn[:, c, :], in0=memT[:, c, :], scalar=g, in1=mrr, op0=ALU.mult, op1=ALU.mult)),
                  reads=[memT[:, c, :], mrr, self.PAR], writes=[memn[:, c, :]])
        for i in range(2):
            t = self.wtiles_cols(wkv, i * 512, 1)[0]
            wv = P.wview(t, KC, 512)
            for mi in range(4):
                pb = P.bank()
                pbm = pb[:, 0:MEM]
                for k in range(KC):
                    lhsT = wv[:, k, mi * 128:(mi + 1) * 128]
                    P.add("pe", (lambda e, pbm=pbm, lhsT=lhsT, k=k: e.matmul(pbm, lhsT=lhsT, rhs=memn[:, k, :], start=(k == 0), stop=(k == KC - 1))),
                          reads=[lhsT, memn[:, k, :]], writes=[pbm])
                self.act_copy(kT[:, i * 4 + mi, :], pbm, scale=1.0 / 16.0)
            P.wrelease(t)
        for i in range(2):
            t = self.wtiles_cols(wkv, D + i * 512, 1)[0]
            wv = P.wview(t, KC, 512)
            for mt in range(2):
                pb = P.bank()
                for k in range(KC):
                    lhsT = memn[:, k, mt * 128:(mt + 1) * 128]
                    rhs = wv[:, k, :]
                    P.add("pe", (lambda e, pb=pb, lhsT=lhsT, rhs=rhs, k=k: e.matmul(pb, lhsT=lhsT, rhs=rhs, start=(k == 0), stop=(k == KC - 1))),
                          reads=[lhsT, rhs], writes=[pb])
                self.act_copy(vtok[:, mt, i * 512:(i + 1) * 512], pb)
            P.wrelease(t)
        for i in range(2):
            t = self.wtiles_cols(T["xa_w_q"][l], i * 512, 1)[0]
            wv = P.wview(t, KC, 512)
            self.lin_fm(wv, KC, 4, lambda k, tb: XN[:, k, tb * TB:(tb + 1) * TB],
                        lambda pb, mi, tb, i=i: self.act_copy(qT[:, i * 4 + mi, tb * TB:(tb + 1) * TB], pb))
            P.wrelease(t)
        it = 0
        for h in range(4):
            for tb in range(NTB):
                ts = slice(tb * TB, (tb + 1) * TB)
                pt = pT[it % 2]
                rd = rden[it % 2]
                it += 1
                for mt in range(2):
                    pb = P.bank()
                    for dc in range(2):
                        lhsT = kT[:, h * 2 + dc, mt * 128:(mt + 1) * 128]
                        rhs = qT[:, h * 2 + dc, ts]
                        P.add("pe", (lambda e, pb=pb, lhsT=lhsT, rhs=rhs, dc=dc: e.matmul(pb, lhsT=lhsT, rhs=rhs, start=(dc == 0), stop=(dc == 1))),
                              reads=[lhsT, rhs], writes=[pb])
                    self.act_copy(pt[:, mt, :], pb, func=AF.Exp)
                pd = P.bank()
                for mt in range(2):
                    rhs = pt[:, mt, :]
                    P.add("pe", (lambda e, pd=pd, rhs=rhs, mt=mt: e.matmul(pd, lhsT=self.ONES, rhs=rhs, start=(mt == 0), stop=(mt == 1))),
                          reads=[self.ONES, rhs], writes=[pd])
                P.add("act", (lambda e, rd=rd, pd=pd: e.activation(out=rd, in_=pd, func=AF.Ln)), reads=[pd], writes=[rd])
                P.add("act", (lambda e, rd=rd: e.activation(out=rd, in_=rd, func=AF.Exp, scale=-1.0)), reads=[rd], writes=[rd])
                for dc in range(2):
                    po = P.bank()
                    for mt in range(2):
                        lhsT = vtok[:, mt, h * 256 + dc * 128: h * 256 + (dc + 1) * 128]
                        rhs = pt[:, mt, :]
                        P.add("pe", (lambda e, po=po, lhsT=lhsT, rhs=rhs, mt=mt: e.matmul(po, lhsT=lhsT, rhs=rhs, start=(mt == 0), stop=(mt == 1))),
                              reads=[lhsT, rhs], writes=[po])
                    dst = XN[:, h * 2 + dc, ts]
                    P.add("dve", (lambda e, dst=dst, po=po, rd=rd: e.tensor_tensor(out=dst, in0=po, in1=rd, op=ALU.mult)),
                          reads=[po, rd], writes=[dst])
        for i in range(2):
            t = self.wtiles_cols(T["xa_w_o"][l], i * 512, 1)[0]
            wv = P.wview(t, KC, 512)
            self.lin_fm(wv, KC, 4, lambda k, tb: XN[:, k, tb * TB:(tb + 1) * TB],
                        lambda pb, mi, tb, i=i: self.add_to_h(pb, i * 4 + mi, tb))
            P.wrelease(t)

    def conv(self, l, j):
        P, T = self.P, self.T
        self.rmsnorm(self.cfg["g_mix"] + l * KC)
        self.salloc_reset()
        yT = self.salloc((KC, S), BF16)
        ch = self.salloc((S + 16,), F32)
        bsb = [self.salloc((TB,), F32) for _ in range(2)]
        csb = [self.salloc((TB,), F32) for _ in range(2)]
        zz = [self.salloc((TB,), F32) for _ in range(2)]
        w_in = T["conv_w_in"][j]
        XN = None if P.dry else self.XN
        dbg = self.cfg.get("dbg", "")
        if not P.dry and "nomemset" not in dbg:
            P.add("dve", lambda e: e.tensor_scalar(out=ch[:, 0:2], in0=self.PAR[:, 0:2], scalar1=0.0, scalar2=None, op0=ALU.mult),
                  reads=[self.PAR], writes=[ch[:, 0:2]])
        it = 0
        for g in range(2):
            tb_ = self.wtiles_cols(w_in, 0 * D + g * 512, 1)[0]
            tc_ = self.wtiles_cols(w_in, 1 * D + g * 512, 1)[0]
            th_ = self.wtiles_cols(w_in, 2 * D + g * 512, 1)[0]
            if P.dry:
                continue
            vb, vc, vh = (P.wview(t, KC, 512) for t in (tb_, tc_, th_))
            for fi in range(4):
                fc = g * 4 + fi
                k0 = self.par(self.cfg["conv_k"] + 0 * KC + fc)
                k1 = self.par(self.cfg["conv_k"] + 1 * KC + fc)
                k2 = self.par(self.cfg["conv_k"] + 2 * KC + fc)
                for tb in range(NTB):
                    ts = slice(tb * TB, (tb + 1) * TB)
                    b_, c_, z_ = bsb[it % 2], csb[it % 2], zz[it % 2]
                    it += 1
                    pbs = []
                    for wv in (vb, vc, vh):
                        pb = P.bank()
                        pbs.append(pb)
                        for k in range(KC):
                            lhsT = wv[:, k, fi * 128:(fi + 1) * 128]
                            rhs = XN[:, k, ts]
                            P.add("pe", (lambda e, pb=pb, lhsT=lhsT, rhs=rhs, k=k: e.matmul(pb, lhsT=lhsT, rhs=rhs, start=(k == 0), stop=(k == KC - 1))),
                                  reads=[lhsT, rhs], writes=[pb])
                    self.act_copy(b_, pbs[0])
                    self.act_copy(c_, pbs[1])
                    chd = ch[:, 2 + tb * TB: 2 + (tb + 1) * TB]
                    P.add("dve", (lambda e, chd=chd, ph=pbs[2], c_=c_: e.tensor_tensor(out=chd, in0=ph, in1=c_, op=ALU.mult)),
                          reads=[pbs[2], c_], writes=[chd])
                    s0 = ch[:, tb * TB: (tb + 1) * TB]
                    s1 = ch[:, 1 + tb * TB: 1 + (tb + 1) * TB]
                    if "noz" in dbg:
                        dst = yT[:, fc, ts]
                        P.add("dve", (lambda e, dst=dst, chd=chd, b_=b_: e.tensor_tensor(out=dst, in0=chd, in1=b_, op=ALU.mult)),
                              reads=[chd, b_], writes=[dst])
                        continue
                    P.add("dve", (lambda e, z_=z_, s0=s0, k0=k0: e.tensor_scalar(out=z_, in0=s0, scalar1=k0, scalar2=None, op0=ALU.mult)),
                          reads=[s0, self.PAR], writes=[z_])
                    P.add("dve", (lambda e, z_=z_, s1=s1, k1=k1: e.scalar_tensor_tensor(out=z_, in0=s1, scalar=k1, in1=z_, op0=ALU.mult, op1=ALU.add)),
                          reads=[s1, z_, self.PAR], writes=[z_])
                    P.add("dve", (lambda e, z_=z_, chd=chd, k2=k2: e.scalar_tensor_tensor(out=z_, in0=chd, scalar=k2, in1=z_, op0=ALU.mult, op1=ALU.add)),
                          reads=[chd, z_, self.PAR], writes=[z_])
                    dst = yT[:, fc, ts]
                    P.add("dve", (lambda e, dst=dst, z_=z_, b_=b_: e.tensor_tensor(out=dst, in0=z_, in1=b_, op=ALU.mult)),
                          reads=[z_, b_], writes=[dst])
            for t in (tb_, tc_, th_):
                P.wrelease(t)
        for i in range(2):
            t = self.wtiles_cols(T["conv_w_out"][j], i * 512, 1)[0]
            if P.dry:
                continue
            wv = P.wview(t, KC, 512)
            self.lin_fm(wv, KC, 4, lambda k, tb: yT[:, k, tb * TB:(tb + 1) * TB],
                        lambda pb, mi, tb, i=i: self.add_to_h(pb, i * 4 + mi, tb))
            P.wrelease(t)

    def mm(self, out, lhsT, rhs, start=True, stop=True):
        self.P.add("pe", (lambda e: e.matmul(out, lhsT=lhsT, rhs=rhs, start=start, stop=stop)), reads=[lhsT, rhs], writes=[out])

    def tr(self, out, in_):
        self.P.add("pe", (lambda e: e.transpose(out, in_, self.IDENT)), reads=[in_, self.IDENT], writes=[out])

    def dve(self, fn, reads, writes):
        self.P.add("dve", fn, reads=reads, writes=writes)

    def gmlp(self, l, j):
        P, T = self.P, self.T
        self.rmsnorm(self.cfg["g_mix"] + l * KC)
        self.salloc_reset()
        HALF = 3072
        vb = self.salloc((4, HALF), BF16)
        gtb = [self.salloc((KC, TB), BF16) for _ in range(2)]
        u_sb = [self.salloc((TB,), F32) for _ in range(2)]
        tmp = [self.salloc((4, 128), F32) for _ in range(2)]
        t1b = [self.salloc((128,), F32) for _ in range(2)]
        st = self.salloc((4, 6, 6), F32)
        mv = self.salloc((4, 2), F32)
        sd = self.salloc((4,), F32)
        rstd = self.salloc((4,), F32)
        ws32 = self.salloc((4, 128), F32)
        msk = self.salloc((128,), F32)
        WmT = self.salloc((4, 128), BF16)
        cb = self.salloc((4, 128), F32)
        bsb = self.salloc((4, 128), F32)
        w_in = T["gmlp_w_in"][j]
        w_out = T["gmlp_w_out"][j].rearrange("(kc p) n -> p kc n", p=128)
        cfg = self.cfg
        XN = None if P.dry else self.XN
        if not P.dry:
            P.add("sp", lambda e: e.dma_start(out=ws32, in_=T["gm_wsT"].rearrange("p (g t) -> p g t", g=4)), writes=[ws32], dma="gm")
            P.add("sp", lambda e: e.dma_start(out=msk, in_=T["gm_mask"][:, :]), writes=[msk], dma="gm")
            P.add("sp", lambda e: e.dma_start(out=bsb, in_=T["gm_bsb"].rearrange("p (g t) -> p g t", g=4)), writes=[bsb], dma="gm")
            pbc = P.bank()
            for g in range(4):
                self.dve(lambda e, g=g: e.tensor_tensor(out=WmT[:, g, :], in0=ws32[:, g, :], in1=msk, op=ALU.mult),
                         [ws32[:, g, :], msk], [WmT[:, g, :]])
                self.mm(pbc[:, g * 128:(g + 1) * 128], self.ONES, WmT[:, g, :])
            self.act_copy(cb, pbc[:, 0:512].rearrange("p (g t) -> p g t", g=4))
        it = 0
        for tb in range(NTB):
            for ct in range(6):
                t = self.wtiles_cols(w_in, HALF + ct * 512, 1)[0]
                if P.dry:
                    continue
                wv = P.wview(t, KC, 512)
                for tt in range(4):
                    pb = P.bank()
                    tok = slice(tb * TB + tt * 128, tb * TB + (tt + 1) * 128)
                    for k in range(KC):
                        self.mm(pb, XN[:, k, tok], wv[:, k, :], start=(k == 0), stop=(k == KC - 1))
                    dst = vb[:, tt, ct * 512:(ct + 1) * 512]
                    self.act_copy(dst, pb, func=AF.Gelu)
                    self.dve(lambda e, dst=dst, tt=tt, ct=ct: e.bn_stats(out=st[:, tt, ct, :], in_=dst), [dst], [st[:, tt, ct, :]])
                P.wrelease(t)
            if not P.dry:
                for tt in range(4):
                    sin_ = st[:, tt, :, :].rearrange("p a b -> p (a b)")
                    self.dve(lambda e, tt=tt, sin_=sin_: e.bn_aggr(out=mv[:, tt, :], in_=sin_), [sin_], [mv[:, tt, :]])
                    P.add("act", (lambda e, tt=tt: e.activation(out=sd[:, tt:tt + 1], in_=mv[:, tt, 1:2], func=AF.Sqrt, bias=self.par(cfg["eps_gn"]), scale=1.0)),
                          reads=[mv[:, tt, 1:2], self.PAR], writes=[sd[:, tt:tt + 1]])
                    self.dve(lambda e, tt=tt: e.reciprocal(out=rstd[:, tt:tt + 1], in_=sd[:, tt:tt + 1]), [sd[:, tt:tt + 1]], [rstd[:, tt:tt + 1]])
                    self.dve(lambda e, tt=tt: e.tensor_scalar(out=vb[:, tt, :], in0=vb[:, tt, :], scalar1=mv[:, tt, 0:1], scalar2=rstd[:, tt:tt + 1], op0=ALU.subtract, op1=ALU.mult),
                             [vb[:, tt, :], mv[:, tt, 0:1], rstd[:, tt:tt + 1]], [vb[:, tt, :]])
            for cg in range(3):
                gt = gtb[cg % 2]
                for cc in range(KC):
                    c = cg * KC + cc
                    g = c // 6
                    if c % 4 == 0:
                        tu = self.wtiles_cols(w_in, (c // 4) * 512, 1)[0]
                        wu = None if P.dry else P.wview(tu, KC, 512)
                    if not P.dry:
                        pbu = P.bank()
                        for k in range(KC):
                            self.mm(pbu, wu[:, k, (c % 4) * 128:(c % 4 + 1) * 128], XN[:, k, tb * TB:(tb + 1) * TB], start=(k == 0), stop=(k == KC - 1))
                        us = u_sb[it % 2]
                        tm = tmp[it % 2]
                        t1 = t1b[it % 2]
                        it += 1
                        self.act_copy(us, pbu, func=AF.Gelu)
                        pbs = P.bank()
                        for tt in range(4):
                            self.mm(pbs[:, tt * 128:(tt + 1) * 128], vb[:, tt, c * 128:(c + 1) * 128], WmT[:, g, :])
                        lnb = self.par(cfg["gm_ln_b"] + c)
                        lng = self.par(cfg["gm_ln_g"] + c)
                        self.dve(lambda e, t1=t1, g=g, lnb=lnb: e.scalar_tensor_tensor(out=t1, in0=cb[:, g, :], scalar=lnb, in1=bsb[:, g, :], op0=ALU.mult, op1=ALU.add),
                                 [cb[:, g, :], bsb[:, g, :], self.PAR], [t1])
                        for tt in range(4):
                            self.dve(lambda e, tm=tm, pbs=pbs, t1=t1, lng=lng, tt=tt: e.scalar_tensor_tensor(out=tm[:, tt, :], in0=pbs[:, tt * 128:(tt + 1) * 128], scalar=lng, in1=t1, op0=ALU.mult, op1=ALU.add),
                                     [pbs[:, tt * 128:(tt + 1) * 128], t1, self.PAR], [tm[:, tt, :]])
                        tmf = tm.rearrange("p a b -> p (a b)")
                        self.dve(lambda e, gt=gt, cc=cc, tmf=tmf, us=us: e.tensor_tensor(out=gt[:, cc, :], in0=tmf, in1=us, op=ALU.mult),
                                 [tmf, us], [gt[:, cc, :]])
                    if c % 4 == 3:
                        P.wrelease(tu)
                for i in range(2):
                    t = P.wtile([(w_out[:, cg * KC:(cg + 1) * KC, i * 512:(i + 1) * 512], 0, KC, 512)])
                    if P.dry:
                        continue
                    wv = P.wview(t, KC, 512)
                    self.lin_fm(wv, KC, 4, lambda k, tb_, gt=gt: gt[:, k, :],
                                lambda pb, mi, tb_, i=i: self.add_to_h(pb, i * 4 + mi, tb_), tbs=[tb])
                    P.wrelease(t)

    def retention(self, l, j):
        P, T = self.P, self.T
        cfg = self.cfg
        self.rmsnorm(cfg["g_mix"] + l * KC)
        self.salloc_reset()
        cosT = self.salloc((S,), F32)
        sinT = self.salloc((S,), F32)
        mark = self._so
        A1 = self.salloc((S,), F32)
        A2 = self.salloc((S,), F32)
        A3 = self.salloc((S,), F32)
        PI = math.pi
        if not P.dry:
            posi = A1.bitcast(I32)
            ni = A3.bitcast(I32)
            P.add("sp", lambda e: e.dma_start(out=posi, in_=T["pos_bc"][:, :]), writes=[posi], dma="pos")
            invf = self.par(cfg["inv_freq"])
            C1 = 6.28125
            C2 = 2.0 * PI - 6.28125
            self.dve(lambda e: e.tensor_scalar(out=A2, in0=posi, scalar1=invf, scalar2=None, op0=ALU.mult), [posi, self.PAR], [A2])
            self.dve(lambda e: e.tensor_scalar(out=ni, in0=A2, scalar1=1.0 / (2.0 * PI), scalar2=None, op0=ALU.mult), [A2], [ni])
            self.dve(lambda e: e.scalar_tensor_tensor(out=A2, in0=ni, scalar=-C1, in1=A2, op0=ALU.mult, op1=ALU.add), [ni, A2], [A2])
            self.dve(lambda e: e.scalar_tensor_tensor(out=A2, in0=ni, scalar=-C2, in1=A2, op0=ALU.mult, op1=ALU.add), [ni, A2], [A2])
            for (shift, dstT) in ((0.0, sinT), (PI / 2.0, cosT)):
                self.dve(lambda e, shift=shift: e.tensor_scalar(out=A1, in0=A2, scalar1=shift, scalar2=None, op0=ALU.add), [A2], [A1])
                self.dve(lambda e: e.tensor_scalar(out=A3, in0=A1, scalar1=PI, scalar2=-2.0 * PI, op0=ALU.is_gt, op1=ALU.mult), [A1], [A3])
                self.dve(lambda e: e.tensor_tensor(out=A1, in0=A1, in1=A3, op=ALU.add), [A1, A3], [A1])
                self.dve(lambda e: e.tensor_scalar(out=A3, in0=A1, scalar1=-PI, scalar2=2.0 * PI, op0=ALU.is_lt, op1=ALU.mult), [A1], [A3])
                self.dve(lambda e: e.tensor_tensor(out=A1, in0=A1, in1=A3, op=ALU.add), [A1, A3], [A1])
                self.dve(lambda e: e.tensor_scalar(out=A1, in0=A1, scalar1=-3.141592, scalar2=3.141592, op0=ALU.max, op1=ALU.min), [A1], [A1])
                self.act_copy(dstT, A1, func=AF.Sin)
        self._so = mark
        qT = self.salloc((2, TB), BF16)
        kT = self.salloc((2, TB), BF16)
        kz = self.salloc((4, 256), BF16)
        v_sb = self.salloc((4, TB), BF16)
        g_sb = self.salloc((4, TB), BF16)
        goT = self.salloc((4, TB), BF16)
        rt = [self.salloc((TB,), F32) for _ in range(4)]
        sTs = [self.salloc((128,), BF16) for _ in range(2)]
        R32 = self.salloc((2, TB), F32)
        Rbf = [self.salloc((2, TB), BF16) for _ in range(2)]
        t1s = [self.salloc((TB,), F32) for _ in range(2)]
        gos = [self.salloc((TB,), BF16) for _ in range(2)]
        st6 = [self.salloc((6,), F32) for _ in range(2)]
        mvs = [self.salloc((2,), F32) for _ in range(2)]
        sds = [self.salloc((1,), F32) for _ in range(2)]
        rss = [self.salloc((1,), F32) for _ in range(2)]
        nms = [self.salloc((1,), F32) for _ in range(2)]
        maskT = self.salloc((4, 128), F32)
        w_in = T["ret_w_in"][j].rearrange("(kc p) n -> p kc n", p=128)
        w_out = T["ret_w_out"][j].rearrange("(ec p) n -> p ec n", p=128)
        XN = None if P.dry else self.XN
        if not P.dry:
            P.add("sp", lambda e: e.dma_start(out=maskT, in_=T["ret_mask"].rearrange("p (h t) -> p h t", h=4)), writes=[maskT], dma="rmask")
        xs = [self.salloc((TB,), F32) for _ in range(4)]
        pending = []

        def flush(n=None):
            k = len(pending) if n is None else min(n, len(pending))
            for _ in range(k):
                pending.pop(0)()

        if not P.dry:
            P.pools = {"proj": [0, 1, 2], "S": [3], "o": [4, 5], "r": [6, 7], "t": [3]}
        for h in range(RET_HEADS):
            gam = 1.0 - 2.0 ** (-5.0 - h)
            decay = gam ** 128
            tqk = P.wtile([(w_in[:, :, h * 256:(h + 1) * 256], 0, KC, 256),
                           (w_in[:, :, D + h * 256: D + (h + 1) * 256], KC * 256, KC, 256)])
            tv = P.wtile([(w_in[:, :, 2 * D + h * 512: 2 * D + (h + 1) * 512], 0, KC, 512)])
            tg = P.wtile([(w_in[:, :, 4 * D + h * 512: 4 * D + (h + 1) * 512], 0, KC, 512)])
            two = P.wtile([(w_out[:, h * 4:(h + 1) * 4, :], 0, 4, D)])
            if P.dry:
                continue
            wq = P.wview(tqk, KC, 256)
            wk = P.wview(tqk, KC, 256, eoff=KC * 256)
            wv_ = P.wview(tv, KC, 512)
            wg = P.wview(tg, KC, 512)
            wo = P.wview(two, 4, D)
            hsrc = self.HT[:, 0, 0:2 * TB].rearrange("p (a b) -> p a b", a=2)
            self.dve(lambda e, hsrc=hsrc: e.tensor_scalar(out=R32, in0=hsrc, scalar1=0.0, scalar2=None, op0=ALU.mult), [hsrc], [R32])
            zcol = self.par(cfg["ret_zeta"] + h)
            ecol = self.par(cfg["ret_epsxi"] + h)
            for tb in range(NTB):
                ts = slice(tb * TB, (tb + 1) * TB)
                xi = 0
                for (wx, XT) in ((wq, qT), (wk, kT)):
                    p1 = P.bank(pool="proj")
                    for k in range(KC):
                        self.mm(p1, wx[:, k, 0:128], XN[:, k, ts], start=(k == 0), stop=(k == KC - 1))
                    x1 = xs[xi]
                    self.act_copy(x1, p1)
                    p2 = P.bank(pool="proj")
                    for k in range(KC):
                        self.mm(p2, wx[:, k, 128:256], XN[:, k, ts], start=(k == 0), stop=(k == KC - 1))
                    x2 = xs[xi + 1]
                    self.act_copy(x2, p2)
                    xi += 2
                    a, b, c, d = rt
                    cs = cosT[:, ts]
                    sn = sinT[:, ts]
                    self.dve(lambda e, a=a, x1=x1, cs=cs: e.tensor_tensor(out=a, in0=x1, in1=cs, op=ALU.mult), [x1, cs], [a])
                    self.dve(lambda e, b=b, x2=x2, sn=sn: e.tensor_tensor(out=b, in0=x2, in1=sn, op=ALU.mult), [x2, sn], [b])
                    self.dve(lambda e, XT=XT, a=a, b=b: e.tensor_tensor(out=XT[:, 0, :], in0=a, in1=b, op=ALU.subtract), [a, b], [XT[:, 0, :]])
                    self.dve(lambda e, c=c, x2=x2, cs=cs: e.tensor_tensor(out=c, in0=x2, in1=cs, op=ALU.mult), [x2, cs], [c])
                    self.dve(lambda e, d=d, x1=x1, sn=sn: e.tensor_tensor(out=d, in0=x1, in1=sn, op=ALU.mult), [x1, sn], [d])
                    self.dve(lambda e, XT=XT, c=c, d=d: e.tensor_tensor(out=XT[:, 1, :], in0=c, in1=d, op=ALU.add), [c, d], [XT[:, 1, :]])
                for c4 in range(4):
                    tok = slice(tb * TB + c4 * 128, tb * TB + (c4 + 1) * 128)
                    pv = P.bank(pool="proj")
                    for k in range(KC):
                        self.mm(pv, XN[:, k, tok], wv_[:, k, :], start=(k == 0), stop=(k == KC - 1))
                    self.act_copy(v_sb[:, c4, :], pv)
                    pg = P.bank(pool="proj")
                    for k in range(KC):
                        self.mm(pg, XN[:, k, tok], wg[:, k, :], start=(k == 0), stop=(k == KC - 1))
                    self.act_copy(g_sb[:, c4, :], pg, func=AF.Silu)
                flush()
                pkt = P.bank(BF16, pool="t")
                for c4 in range(4):
                    for dc in range(2):
                        self.tr(pkt[:, c4 * 256 + dc * 128: c4 * 256 + (dc + 1) * 128], kT[:, dc, c4 * 128:(c4 + 1) * 128])
                kzf = kz.rearrange("p a b -> p (a b)")
                P.add("act", (lambda e, kzf=kzf, pkt=pkt, zcol=zcol: e.activation(out=kzf, in_=pkt, func=AF.Identity, scale=zcol)),
                      reads=[pkt, self.PAR], writes=[kzf])

                pos = {}

                def stA(c4, tb=tb, h=h, decay=decay):
                    ci = tb * 4 + c4
                    cols = slice(c4 * 128, (c4 + 1) * 128)
                    pS = P.bank(pool="S")
                    pSs = pS[:, 0:128]
                    for dc in range(2):
                        self.mm(pSs, kT[:, dc, cols], qT[:, dc, cols], start=(dc == 0), stop=(dc == 1))
                    sT = sTs[ci % 2]
                    self.dve(lambda e, sT=sT, pSs=pSs: e.tensor_tensor(out=sT, in0=pSs, in1=maskT[:, h, :], op=ALU.mult), [pSs, maskT[:, h, :]], [sT])
                    if ci < 15:
                        Rn = Rbf[ci % 2]
                        for dc in range(2):
                            pr = P.bank(pool="r")
                            self.mm(pr, kz[:, c4, dc * 128:(dc + 1) * 128], v_sb[:, c4, :])
                            self.dve(lambda e, dc=dc, pr=pr: e.scalar_tensor_tensor(out=R32[:, dc, :], in0=R32[:, dc, :], scalar=decay, in1=pr, op0=ALU.mult, op1=ALU.add),
                                     [R32[:, dc, :], pr], [R32[:, dc, :]])
                            self.act_copy(Rn[:, dc, :], R32[:, dc, :])

                def stB(c4, tb=tb, ecol=ecol):
                    ci = tb * 4 + c4
                    cols = slice(c4 * 128, (c4 + 1) * 128)
                    sT = sTs[ci % 2]
                    po = P.bank(pool="o")
                    pos[c4] = po
                    self.mm(po, sT, v_sb[:, c4, :], start=True, stop=(ci == 0))
                    if ci > 0:
                        Rb = Rbf[(ci - 1) % 2]
                        for dc in range(2):
                            self.mm(po, qT[:, dc, cols], Rb[:, dc, :], start=False, stop=(dc == 1))
                    s6, mv_, sd_ = st6[ci % 2], mvs[ci % 2], sds[ci % 2]
                    self.dve(lambda e: e.bn_stats(out=s6, in_=po), [po], [s6])
                    self.dve(lambda e: e.bn_aggr(out=mv_, in_=s6), [s6], [mv_])
                    P.add("act", (lambda e: e.activation(out=sd_, in_=mv_[:, 1:2], func=AF.Sqrt, bias=ecol, scale=1.0)),
                          reads=[mv_[:, 1:2], self.PAR], writes=[sd_])

                def stC(c4, tb=tb):
                    ci = tb * 4 + c4
                    po = pos[c4]
                    mv_, sd_, rs_ = mvs[ci % 2], sds[ci % 2], rss[ci % 2]
                    t1, go = t1s[ci % 2], gos[ci % 2]
                    self.dve(lambda e: e.reciprocal(out=rs_, in_=sd_), [sd_], [rs_])
                    self.dve(lambda e: e.tensor_scalar(out=t1, in0=po, scalar1=mv_[:, 0:1], scalar2=rs_, op0=ALU.subtract, op1=ALU.mult),
                             [po, mv_[:, 0:1], rs_], [t1])
                    self.dve(lambda e: e.tensor_tensor(out=go, in0=t1, in1=g_sb[:, c4, :], op=ALU.mult), [t1, g_sb[:, c4, :]], [go])

                def stD(c4, tb=tb, h=h):
                    ci = tb * 4 + c4
                    go = gos[ci % 2]
                    pgt = P.bank(BF16, pool="t")
                    for ec in range(4):
                        self.tr(pgt[:, ec * 128:(ec + 1) * 128], go[:, ec * 128:(ec + 1) * 128])
                    for ec in range(4):
                        gcol = self.par(cfg["ret_gn_g"] + j * 16 + h * 4 + ec)
                        dst = goT[:, ec, c4 * 128:(c4 + 1) * 128]
                        src = pgt[:, ec * 128:(ec + 1) * 128]
                        P.add("act", (lambda e, dst=dst, src=src, gcol=gcol: e.activation(out=dst, in_=src, func=AF.Identity, scale=gcol)),
                              reads=[src, self.PAR], writes=[dst])

                for step in range(4 + 3):
                    if 0 <= step - 3 < 4:
                        stD(step - 3)
                    if 0 <= step - 2 < 4:
                        stC(step - 2)
                    if 0 <= step - 1 < 4:
                        stB(step - 1)
                    if step < 4:
                        stA(step)

                def wout(tb=tb, wo=wo):
                    for mi in range(KC):
                        pb = P.bank(pool="proj")
                        for k in range(4):
                            self.mm(pb, wo[:, k, mi * 128:(mi + 1) * 128], goT[:, k, :], start=(k == 0), stop=(k == 3))
                        self.add_to_h(pb, mi, tb)
                pending.append(wout)
            flush()
            for t in (tqk, tv, tg, two):
                P.wrelease(t)
        if not P.dry:
            P.pools = {}

    def epilogue(self):
        P, T = self.P, self.T
        self.rmsnorm(self.cfg["g_final"], inplace=True)
        if P.dry:
            return
        oT = T["outT"].rearrange("(c p) t -> p c t", p=128)
        for c in range(KC):
            src = self.HT[:, c, :]
            P.add("sp", (lambda e, src=src, c=c: e.dma_start(out=oT[:, c, :], in_=src)), reads=[src], dma="out")


def make_params(inputs, cfg_only=False):
    cols = []
    cfg = {}

    def addvec(name, v):
        n = v.shape[0] // 128
        cfg_name_start = sum(c.shape[1] for c in cols)
        cols.append(np.ascontiguousarray(v.reshape(n, 128).T))
        return cfg_name_start

    def addconst(val):
        st = sum(c.shape[1] for c in cols)
        cols.append(np.full((128, 1), val, np.float32))
        return st

    cfg["g_mix"] = addvec("g_mix", inputs["norm_mix_g"].reshape(-1))
    cfg["g_xa"] = addvec("g_xa", inputs["norm_xa_g"].reshape(-1))
    cfg["g_mem"] = addvec("g_mem", inputs["norm_mem_g"].reshape(-1))
    cfg["g_ffn"] = addvec("g_ffn", inputs["norm_ffn_g"].reshape(-1))
    cfg["g_final"] = addvec("g_final", inputs["norm_f_g"].reshape(-1))
    cfg["eps_norm"] = addconst(NORM_EPS)
    cfg["conv_k"] = addvec("conv_k", inputs["conv_k"][0].reshape(-1))
    cfg["eps_gn"] = addconst(GN_EPS)
    cfg["gm_ln_g"] = addvec("gm_ln_g", inputs["gmlp_ln_g"][0])
    cfg["gm_ln_b"] = addvec("gm_ln_b", inputs["gmlp_ln_b"][0])
    cfg["ret_gn_g"] = addvec("ret_gn_g", inputs["ret_gn_g"].reshape(-1))
    half = 128
    inv = (10000.0 ** (-np.arange(half, dtype=np.float32) / half)).astype(np.float32)
    cfg["inv_freq"] = addvec("inv_freq", inv)
    idx = np.arange(128, dtype=np.float64)
    zs, es = [], []
    for h in range(RET_HEADS):
        gam = 1.0 - 2.0 ** (-5.0 - h)
        zs.append((gam ** (127.0 - idx)) / 16.0)
        es.append(GN_EPS / (gam ** (2.0 * (idx + 1.0))))
    cfg["ret_zeta"] = sum(c.shape[1] for c in cols)
    cols.append(np.stack(zs, 1).astype(np.float32))
    cfg["ret_epsxi"] = sum(c.shape[1] for c in cols)
    cols.append(np.stack(es, 1).astype(np.float32))
    par = np.concatenate(cols, axis=1).astype(np.float32)
    cfg["npar"] = par.shape[1]
    return par, cfg


def ret_mask_const():
    sidx = np.arange(128, dtype=np.float64)[:, None]
    tidx = np.arange(128, dtype=np.float64)[None, :]
    out = np.zeros((128, RET_HEADS, 128), np.float32)
    for h in range(RET_HEADS):
        gam = 1.0 - 2.0 ** (-5.0 - h)
        out[:, h, :] = np.where(tidx >= sidx, gam ** (-sidx - 1.0) / 16.0, 0.0)
    return np.ascontiguousarray(out.reshape(128, RET_HEADS * 128))


def build_program(cfg, stages):
    nc = bass.Bass("TRN2", target_bir_lowering=False)
    T = {}

    def din(name, shape, dt=F32):
        T[name] = nc.dram_tensor(name, list(shape), dt, kind="ExternalInput").ap()

    din("xT", (D, S))
    din("params", (128, cfg["npar"]))
    din("ones_bf", (128, 128), BF16)
    din("ident_bf", (128, 128), BF16)
    din("ffn_w1", (DEPTH, D, 4 * D))
    din("ffn_w2", (DEPTH, 4 * D, D))
    din("memT", (D, MEM))
    din("xa_w_q", (DEPTH, D, D))
    din("xa_w_kv", (DEPTH, D, 2 * D))
    din("xa_w_o", (DEPTH, D, D))
    din("conv_w_in", (1, D, 3 * D))
    din("conv_w_out", (1, D, D))
    din("gmlp_w_in", (1, D, 6 * D))
    din("gmlp_w_out", (1, 3 * D, D))
    din("gm_wsT", (128, 512))
    din("gm_mask", (128, 128))
    din("gm_bsb", (128, 512))
    din("ret_w_in", (2, D, 6 * D))
    din("ret_w_out", (2, 2 * D, D))
    din("ret_mask", (128, 512))
    din("pos_bc", (128, S), I32)
    T["outT"] = nc.dram_tensor("outT", [D, S], F32, kind="ExternalOutput").ap()

    def program(P):
        B = Builder(P, T, cfg)
        B.prologue()
        stages(B)
        B.epilogue()

    Pd = Prog(nc, None, None, dry=True)
    program(Pd)
    sched = Pd.sched
    import contextlib
    with contextlib.ExitStack() as st:
        sb = st.enter_context(nc.sbuf_tensor("sb", [128, 212480], U8))
        ps = st.enter_context(nc.psum_tensor("ps", [128, 8, 2048], U8))
        P = Prog(nc, sb, ps, dry=False, sched=sched)
        program(P)
        P.emit()
    return nc


def default_stages(B):
    for l in range(DEPTH):
        kind, j = l % 3, l // 3
        if kind == 0:
            B.retention(l, j)
        elif kind == 1:
            B.gmlp(l, j)
        else:
            B.conv(l, j)
        B.xattn(l)
        B.ffn(l)


def host_inputs(inputs, b, par):
    import ml_dtypes
    m = {
        "xT": np.ascontiguousarray(inputs["x"][b].T),
        "params": par,
        "ones_bf": np.ones((128, 128), ml_dtypes.bfloat16),
        "ident_bf": np.eye(128, dtype=np.float32).astype(ml_dtypes.bfloat16),
        "ffn_w1": inputs["ffn_w1"],
        "ffn_w2": inputs["ffn_w2"],
        "memT": np.ascontiguousarray(inputs["mem"][b].T),
        "xa_w_q": inputs["xa_w_q"], "xa_w_kv": inputs["xa_w_kv"], "xa_w_o": inputs["xa_w_o"],
        "conv_w_in": inputs["conv_w_in"], "conv_w_out": inputs["conv_w_out"],
        "gmlp_w_in": inputs["gmlp_w_in"], "gmlp_w_out": inputs["gmlp_w_out"],
        "gm_wsT": np.ascontiguousarray(inputs["gmlp_w_s"][0].transpose(2, 0, 1).reshape(128, 512)),
        "gm_mask": np.triu(np.ones((128, 128), np.float32)),
        "gm_bsb": np.ascontiguousarray(np.broadcast_to(inputs["gmlp_b_s"][0].reshape(1, 512), (128, 512))),
        "ret_w_in": inputs["ret_w_in"], "ret_w_out": inputs["ret_w_out"],
        "ret_mask": ret_mask_const(),
        "pos_bc": np.ascontiguousarray(np.broadcast_to(inputs["positions"][b][None, :], (128, S))).astype(np.int32),
    }
    return m


def kernel(**inputs):
    inputs = {k: np.asarray(v) for k, v in inputs.items()}
    par, cfg = make_params(inputs)
    nc = build_program(cfg, default_stages)
    in_maps = [host_inputs(inputs, b, par) for b in range(8)]
    res = run_bass_kernel_spmd(nc, in_maps, core_ids=list(range(8)))
    out = np.stack([np.ascontiguousarray(r["outT"].T) for r in res.results], axis=0)
    return out.astype(np.float32)
```
